# Optimizing a Trainium2 kernel written in Bass

```python
import math
import jax, jax.numpy as jnp
from jax import lax
import numpy as np

D_MODEL = 1024
BATCH = 4
SEQ = 4096
DEPTH = 4

GRID_W = 64
CTX_LEN = 256
N_MIXERS = 3
Q_BLOCK = 128
NORM_EPS = 1e-6
ROPE_THETA = 10000.0
NEG_INF = -1e30

A_HEADS = 16
A_KV_HEADS = 4
A_REP = A_HEADS // A_KV_HEADS
HEAD_DIM = 64
B_HEADS = 16
WIN_R = 8
WIN_C = 16
C_HEADS = 16
Q_LORA = 384
KV_LORA = 256
NOPE_DIM = 64
ROPE_DIM = 32
V_DIM = 64
D_FF = 2816
N_EXPERTS = 8
TOP_K = 2
D_FF_EXPERT = 3584

kernel_name = 'hybrid_dit_gqa_natten_mla_moe'


def _rmsnorm(x, g):
    xf = x.astype(jnp.float32)
    y = xf * lax.rsqrt(jnp.mean(xf * xf, axis=-1, keepdims=True) + NORM_EPS)
    return (y * g.astype(jnp.float32)).astype(x.dtype)


def _modulate(h, shift, scale):
    return h * (1 + scale) + shift


def _adaln(cvec, w_mod, b_mod):
    return jnp.split(jax.nn.silu(cvec) @ w_mod + b_mod, 6, axis=-1)


def _rope_1d(x, pos):
    half = x.shape[-1] // 2
    inv_freq = jnp.exp(-math.log(ROPE_THETA) * jnp.arange(half, dtype=jnp.float32) / half)
    ang = pos.astype(jnp.float32)[:, None] * inv_freq[None, :]
    cos, sin = jnp.cos(ang), jnp.sin(ang)
    xf = x.astype(jnp.float32)
    x1, x2 = xf[..., :half], xf[..., half:]
    return jnp.concatenate([x1 * cos - x2 * sin, x1 * sin + x2 * cos], axis=-1).astype(x.dtype)


def _rope_2d(x, rows, cols):
    h = x.shape[-1] // 2
    return jnp.concatenate([_rope_1d(x[..., :h], rows), _rope_1d(x[..., h:], cols)], axis=-1)


def _block_attention(q, k, v):
    B, G, R, T, dk = q.shape
    dv = v.shape[-1]
    nb = T // Q_BLOCK
    scale = dk ** -0.5
    qb = jnp.moveaxis(q.reshape(B, G, R, nb, Q_BLOCK, dk), 3, 0)

    def one_block(qi):
        s = jnp.einsum('bgrqd,bgkd->bgrqk', qi, k, preferred_element_type=jnp.float32) * scale
        p = jax.nn.softmax(s, axis=-1)
        return jnp.einsum('bgrqk,bgkd->bgrqd', p.astype(v.dtype), v)

    o = lax.map(one_block, qb)
    return jnp.moveaxis(o, 0, 3).reshape(B, G, R, T, dv)


def _gqa_project(h, w_qkv, q_gain, k_gain):
    B, N, _ = h.shape
    q, k, v = jnp.split(h @ w_qkv, [A_HEADS * HEAD_DIM, (A_HEADS + A_KV_HEADS) * HEAD_DIM], axis=-1)
    q = _rmsnorm(q.reshape(B, N, A_KV_HEADS, A_REP, HEAD_DIM), q_gain).transpose(0, 2, 3, 1, 4)
    k = _rmsnorm(k.reshape(B, N, A_KV_HEADS, HEAD_DIM), k_gain).transpose(0, 2, 1, 3)
    v = v.reshape(B, N, A_KV_HEADS, HEAD_DIM).transpose(0, 2, 1, 3)
    return q, k, v


def _gqa_mixer(h_lat, h_ctx, rows, cols, need_ctx, w_qkv, q_gain, k_gain, w_o):
    B, T, _ = h_lat.shape
    q_l, k_l, v_l = _gqa_project(h_lat, w_qkv, q_gain, k_gain)
    q_l = _rope_2d(q_l, rows, cols)
    k_l = _rope_2d(k_l, rows, cols)
    q_c, k_c, v_c = _gqa_project(h_ctx, w_qkv, q_gain, k_gain)
    k_all = jnp.concatenate([k_c, k_l], axis=2)
    v_all = jnp.concatenate([v_c, v_l], axis=2)
    o_l = _block_attention(q_l, k_all, v_all)
    o_l = o_l.transpose(0, 3, 1, 2, 4).reshape(B, T, A_HEADS * HEAD_DIM) @ w_o
    o_c = None
    if need_ctx:
        n_ctx = h_ctx.shape[1]
        o_c = _block_attention(q_c, k_c, v_c)
        o_c = o_c.transpose(0, 3, 1, 2, 4).reshape(B, n_ctx, A_HEADS * HEAD_DIM) @ w_o
    return o_l, o_c


def _mha_project(h, w_qkv, q_gain, k_gain):
    B, N, _ = h.shape
    q, k, v = jnp.split(h @ w_qkv, 3, axis=-1)
    q = _rmsnorm(q.reshape(B, N, B_HEADS, HEAD_DIM), q_gain).transpose(0, 2, 1, 3)
    k = _rmsnorm(k.reshape(B, N, B_HEADS, HEAD_DIM), k_gain).transpose(0, 2, 1, 3)
    v = v.reshape(B, N, B_HEADS, HEAD_DIM).transpose(0, 2, 1, 3)
    return q, k, v


def _natten_mixer(h_lat, h_ctx, need_ctx, w_qkv, q_gain, k_gain, rel_bias, w_o):
    B, T, _ = h_lat.shape
    rows_n = T // GRID_W
    wr = min(WIN_R, rows_n)
    scale = HEAD_DIM ** -0.5
    q_l, k_l, v_l = _mha_project(h_lat, w_qkv, q_gain, k_gain)
    q_c, k_c, v_c = _mha_project(h_ctx, w_qkv, q_gain, k_gain)
    n_ctx = k_c.shape[2]
    kg = k_l.reshape(B, B_HEADS, rows_n, GRID_W, HEAD_DIM)
    vg = v_l.reshape(B, B_HEADS, rows_n, GRID_W, HEAD_DIM)
    qg = jnp.moveaxis(q_l.reshape(B, B_HEADS, rows_n, GRID_W, HEAD_DIM), 2, 0)
    col = jnp.arange(GRID_W, dtype=jnp.int32)
    c0 = jnp.clip(col - WIN_C // 2, 0, GRID_W - WIN_C)
    col_ok = (col[None, :] >= c0[:, None]) & (col[None, :] < c0[:, None] + WIN_C)
    col_idx = jnp.clip(col[None, :] - col[:, None] + WIN_C - 1, 0, 2 * WIN_C - 2)
    band_ok = jnp.tile(col_ok, (1, wr))

    def one_row(args):
        r, qr = args
        r0 = jnp.clip(r - wr // 2, 0, rows_n - wr)
        kr = lax.dynamic_slice_in_dim(kg, r0, wr, axis=2).reshape(B, B_HEADS, wr * GRID_W, HEAD_DIM)
        vr = lax.dynamic_slice_in_dim(vg, r0, wr, axis=2).reshape(B, B_HEADS, wr * GRID_W, HEAD_DIM)
        row_idx = r0 + jnp.arange(wr, dtype=jnp.int32) - r + WIN_R - 1
        bias = rel_bias[:, row_idx][:, :, col_idx]
        bias = bias.transpose(0, 2, 1, 3).reshape(B_HEADS, GRID_W, wr * GRID_W).astype(jnp.float32)
        s_l = jnp.einsum('bhqd,bhkd->bhqk', qr, kr, preferred_element_type=jnp.float32) * scale + bias
        s_l = jnp.where(band_ok, s_l, NEG_INF)
        s_c = jnp.einsum('bhqd,bhkd->bhqk', qr, k_c, preferred_element_type=jnp.float32) * scale
        p = jax.nn.softmax(jnp.concatenate([s_c, s_l], axis=-1), axis=-1).astype(v_l.dtype)
        return (jnp.einsum('bhqk,bhkd->bhqd', p[..., :n_ctx], v_c)
                + jnp.einsum('bhqk,bhkd->bhqd', p[..., n_ctx:], vr))

    o = lax.map(one_row, (jnp.arange(rows_n, dtype=jnp.int32), qg))
    o_l = o.transpose(1, 0, 3, 2, 4).reshape(B, T, B_HEADS * HEAD_DIM) @ w_o
    o_c = None
    if need_ctx:
        o_c = _block_attention(q_c[:, :, None], k_c, v_c)[:, :, 0]
        o_c = o_c.transpose(0, 2, 1, 3).reshape(B, n_ctx, B_HEADS * HEAD_DIM) @ w_o
    return o_l, o_c


def _mla_project(h, w_in, g_dq, g_dkv, w_uq, w_ukv, q_gain, k_gain):
    B, N, _ = h.shape
    cq, ckv, kr = jnp.split(h @ w_in, [Q_LORA, Q_LORA + KV_LORA], axis=-1)
    q = (_rmsnorm(cq, g_dq) @ w_uq).reshape(B, N, C_HEADS, NOPE_DIM + ROPE_DIM)
    kv = (_rmsnorm(ckv, g_dkv) @ w_ukv).reshape(B, N, C_HEADS, NOPE_DIM + V_DIM)
    k_nope, v = jnp.split(kv, [NOPE_DIM], axis=-1)
    q_nope = _rmsnorm(q[..., :NOPE_DIM], q_gain[:NOPE_DIM]).transpose(0, 2, 1, 3)
    q_rope = _rmsnorm(q[..., NOPE_DIM:], q_gain[NOPE_DIM:]).transpose(0, 2, 1, 3)
    k_nope = _rmsnorm(k_nope, k_gain[:NOPE_DIM]).transpose(0, 2, 1, 3)
    k_rope = _rmsnorm(kr, k_gain[NOPE_DIM:])
    return q_nope, q_rope, k_nope, k_rope, v.transpose(0, 2, 1, 3)


def _mla_assemble(q_nope, q_rope, k_nope, k_rope):
    q = jnp.concatenate([q_nope, q_rope], axis=-1)[:, :, None]
    k_rope_h = jnp.broadcast_to(k_rope[:, None], k_nope.shape[:-1] + (ROPE_DIM,))
    k = jnp.concatenate([k_nope, k_rope_h], axis=-1)
    return q, k


def _mla_mixer(h_lat, h_ctx, rows, cols, need_ctx, w_in, g_dq, g_dkv, w_uq, w_ukv, q_gain, k_gain, w_o):
    B, T, _ = h_lat.shape
    qn_l, qr_l, kn_l, kr_l, v_l = _mla_project(h_lat, w_in, g_dq, g_dkv, w_uq, w_ukv, q_gain, k_gain)
    qn_c, qr_c, kn_c, kr_c, v_c = _mla_project(h_ctx, w_in, g_dq, g_dkv, w_uq, w_ukv, q_gain, k_gain)
    q_l, k_l = _mla_assemble(qn_l, _rope_2d(qr_l, rows, cols), kn_l, _rope_2d(kr_l, rows, cols))
    q_c, k_c = _mla_assemble(qn_c, qr_c, kn_c, kr_c)
    k_all = jnp.concatenate([k_c, k_l], axis=2)
    v_all = jnp.concatenate([v_c, v_l], axis=2)
    o_l = _block_attention(q_l, k_all, v_all)[:, :, 0]
    o_l = o_l.transpose(0, 2, 1, 3).reshape(B, T, C_HEADS * V_DIM) @ w_o
    o_c = None
    if need_ctx:
        n_ctx = h_ctx.shape[1]
        o_c = _block_attention(q_c, k_c, v_c)[:, :, 0]
        o_c = o_c.transpose(0, 2, 1, 3).reshape(B, n_ctx, C_HEADS * V_DIM) @ w_o
    return o_l, o_c


def _swiglu(h, w_gu, w_down):
    g, u = jnp.split(h @ w_gu, 2, axis=-1)
    return (jax.nn.silu(g) * u) @ w_down


def _moe(h, w_router, we_gu, we_down):
    shape = h.shape
    t = h.reshape(-1, shape[-1])
    logits = jnp.dot(t, w_router, preferred_element_type=jnp.float32)
    top_val, top_idx = lax.top_k(logits, TOP_K)
    top_w = jax.nn.softmax(top_val, axis=-1)
    combine = jnp.einsum('nk,nke->ne', top_w,
                         jax.nn.one_hot(top_idx, N_EXPERTS, dtype=jnp.float32)).astype(h.dtype)
    out = jnp.zeros_like(t)
    for e in range(N_EXPERTS):
        out = out + combine[:, e:e + 1] * _swiglu(t, we_gu[e], we_down[e])
    return out.reshape(shape)


def setup_inputs(seed: int = 0) -> dict:
    key = jax.random.key(seed)
    keys = jax.random.split(key, 96)
    counter = iter(range(96))

    def rnd(shape, scale):
        return jax.random.normal(keys[next(counter)], shape, jnp.float32) * scale

    def gain(n):
        return 1.0 + rnd((n,), 0.05)

    D = D_MODEL
    inp = {}
    inp['x'] = rnd((BATCH, SEQ, D), 1.0)
    inp['c'] = rnd((BATCH, D), 1.0)
    inp['ctx'] = rnd((BATCH, CTX_LEN, D), 1.0)
    inp['c_ctx'] = rnd((D,), 1.0)
    for i in range(DEPTH):
        p = 'l%d_' % i
        inp[p + 'w_mod'] = rnd((D, 6 * D), 0.5 * D ** -0.5)
        inp[p + 'b_mod'] = rnd((6 * D,), 0.02)
        inp[p + 'g_mix'] = gain(D)
        inp[p + 'g_ffn'] = gain(D)
        kind = i % N_MIXERS
        if kind == 0:
            inp[p + 'w_qkv'] = rnd((D, (A_HEADS + 2 * A_KV_HEADS) * HEAD_DIM), D ** -0.5)
            inp[p + 'q_gain'] = gain(HEAD_DIM)
            inp[p + 'k_gain'] = gain(HEAD_DIM)
            inp[p + 'w_o'] = rnd((A_HEADS * HEAD_DIM, D), (A_HEADS * HEAD_DIM) ** -0.5)
        elif kind == 1:
            inp[p + 'w_qkv'] = rnd((D, 3 * B_HEADS * HEAD_DIM), D ** -0.5)
            inp[p + 'q_gain'] = gain(HEAD_DIM)
            inp[p + 'k_gain'] = gain(HEAD_DIM)
            inp[p + 'rel_bias'] = rnd((B_HEADS, 2 * WIN_R - 1, 2 * WIN_C - 1), 0.2)
            inp[p + 'w_o'] = rnd((B_HEADS * HEAD_DIM, D), (B_HEADS * HEAD_DIM) ** -0.5)
        else:
            inp[p + 'w_in'] = rnd((D, Q_LORA + KV_LORA + ROPE_DIM), D ** -0.5)
            inp[p + 'g_dq'] = gain(Q_LORA)
            inp[p + 'g_dkv'] = gain(KV_LORA)
            inp[p + 'w_uq'] = rnd((Q_LORA, C_HEADS * (NOPE_DIM + ROPE_DIM)), Q_LORA ** -0.5)
            inp[p + 'w_ukv'] = rnd((KV_LORA, C_HEADS * (NOPE_DIM + V_DIM)), KV_LORA ** -0.5)
            inp[p + 'q_gain'] = gain(NOPE_DIM + ROPE_DIM)
            inp[p + 'k_gain'] = gain(NOPE_DIM + ROPE_DIM)
            inp[p + 'w_o'] = rnd((C_HEADS * V_DIM, D), (C_HEADS * V_DIM) ** -0.5)
        if i % 2 == 0:
            inp[p + 'w_gu'] = rnd((D, 2 * D_FF), D ** -0.5)
            inp[p + 'w_down'] = rnd((D_FF, D), D_FF ** -0.5)
        else:
            inp[p + 'w_router'] = rnd((D, N_EXPERTS), D ** -0.5)
            inp[p + 'we_gu'] = rnd((N_EXPERTS, D, 2 * D_FF_EXPERT), D ** -0.5)
            inp[p + 'we_down'] = rnd((N_EXPERTS, D_FF_EXPERT, D), D_FF_EXPERT ** -0.5)
    return inp


def reference(x, c, ctx, c_ctx,
              l0_w_mod, l0_b_mod, l0_g_mix, l0_g_ffn, l0_w_qkv, l0_q_gain, l0_k_gain, l0_w_o, l0_w_gu, l0_w_down,
              l1_w_mod, l1_b_mod, l1_g_mix, l1_g_ffn, l1_w_qkv, l1_q_gain, l1_k_gain, l1_rel_bias, l1_w_o,
              l1_w_router, l1_we_gu, l1_we_down,
              l2_w_mod, l2_b_mod, l2_g_mix, l2_g_ffn, l2_w_in, l2_g_dq, l2_g_dkv, l2_w_uq, l2_w_ukv, l2_q_gain,
              l2_k_gain, l2_w_o, l2_w_gu, l2_w_down,
              l3_w_mod, l3_b_mod, l3_g_mix, l3_g_ffn, l3_w_qkv, l3_q_gain, l3_k_gain, l3_w_o,
              l3_w_router, l3_we_gu, l3_we_down):
    layers = [
        dict(w_mod=l0_w_mod, b_mod=l0_b_mod, g_mix=l0_g_mix, g_ffn=l0_g_ffn,
             mix=dict(w_qkv=l0_w_qkv, q_gain=l0_q_gain, k_gain=l0_k_gain, w_o=l0_w_o),
             ffn=dict(w_gu=l0_w_gu, w_down=l0_w_down)),
        dict(w_mod=l1_w_mod, b_mod=l1_b_mod, g_mix=l1_g_mix, g_ffn=l1_g_ffn,
             mix=dict(w_qkv=l1_w_qkv, q_gain=l1_q_gain, k_gain=l1_k_gain, rel_bias=l1_rel_bias, w_o=l1_w_o),
             ffn=dict(w_router=l1_w_router, we_gu=l1_we_gu, we_down=l1_we_down)),
        dict(w_mod=l2_w_mod, b_mod=l2_b_mod, g_mix=l2_g_mix, g_ffn=l2_g_ffn,
             mix=dict(w_in=l2_w_in, g_dq=l2_g_dq, g_dkv=l2_g_dkv, w_uq=l2_w_uq, w_ukv=l2_w_ukv,
                      q_gain=l2_q_gain, k_gain=l2_k_gain, w_o=l2_w_o),
             ffn=dict(w_gu=l2_w_gu, w_down=l2_w_down)),
        dict(w_mod=l3_w_mod, b_mod=l3_b_mod, g_mix=l3_g_mix, g_ffn=l3_g_ffn,
             mix=dict(w_qkv=l3_w_qkv, q_gain=l3_q_gain, k_gain=l3_k_gain, w_o=l3_w_o),
             ffn=dict(w_router=l3_w_router, we_gu=l3_we_gu, we_down=l3_we_down)),
    ]
    T = x.shape[1]
    pos = jnp.arange(T, dtype=jnp.int32)
    rows = pos // GRID_W
    cols = pos % GRID_W
    x_lat, x_ctx = x, ctx
    for i in range(DEPTH):
        p = layers[i]
        need_ctx = i < DEPTH - 1
        sh1, sc1, g1, sh2, sc2, g2 = [m[:, None, :] for m in _adaln(c, p['w_mod'], p['b_mod'])]
        csh1, csc1, cg1, csh2, csc2, cg2 = _adaln(c_ctx, p['w_mod'], p['b_mod'])
        h_lat = _modulate(_rmsnorm(x_lat, p['g_mix']), sh1, sc1)
        h_ctx = _modulate(_rmsnorm(x_ctx, p['g_mix']), csh1, csc1)
        kind = i % N_MIXERS
        if kind == 0:
            o_lat, o_ctx = _gqa_mixer(h_lat, h_ctx, rows, cols, need_ctx, **p['mix'])
        elif kind == 1:
            o_lat, o_ctx = _natten_mixer(h_lat, h_ctx, need_ctx, **p['mix'])
        else:
            o_lat, o_ctx = _mla_mixer(h_lat, h_ctx, rows, cols, need_ctx, **p['mix'])
        x_lat = x_lat + g1 * o_lat
        channel = _swiglu if i % 2 == 0 else _moe
        h2_lat = _modulate(_rmsnorm(x_lat, p['g_ffn']), sh2, sc2)
        if need_ctx:
            x_ctx = x_ctx + cg1 * o_ctx
            h2_ctx = _modulate(_rmsnorm(x_ctx, p['g_ffn']), csh2, csc2)
            n_ctx = x_ctx.shape[1]
            f = channel(jnp.concatenate([h2_ctx, h2_lat], axis=1), **p['ffn'])
            x_ctx = x_ctx + cg2 * f[:, :n_ctx]
            x_lat = x_lat + g2 * f[:, n_ctx:]
        else:
            x_lat = x_lat + g2 * channel(h2_lat, **p['ffn'])
    return x_lat
```

```python
import contextlib
import math
import numpy as np
import ml_dtypes
import concourse.bass as bass
import concourse.mybir as mybir
from concourse.bass_utils import run_bass_kernel_spmd

F32 = mybir.dt.float32
BF16 = mybir.dt.bfloat16
AF = mybir.ActivationFunctionType
ALU = mybir.AluOpType
AX = mybir.AxisListType

D = 1024
NC = 8
TL = 2048
TC = 256
NT = TL + TC
EPS = 1e-6
GROUPS = [(0, 256, 1)] + [(256 + 512 * i, 512, 0) for i in range(4)]
D_FF = 2816
D_FFE = 3584
NEXP = 8


class Prog:
    ENGS = ("pe", "act", "dve", "pool", "sp")

    def __init__(self, nc):
        self.nc = nc
        self.ops = {e: [] for e in self.ENGS}
        self.count = {}
        self.known = {e: {} for e in self.ENGS}
        self.lastw = {}
        self.readers = {}
        self.stack = contextlib.ExitStack()
        self.nbuf = 0
        self.psum = []
        self.psum_rr = {}
        self.arena = None
        self.dma_rr = {}
        self.off = 0
        self.peak = 0

    ARENA_WORDS = 184 * 256
    NDS = 8

    def sb(self, shape, dt, name=None):
        if self.arena is None:
            self.arena = self.stack.enter_context(self.nc.sbuf_tensor("arena", [128, self.ARENA_WORDS], F32))
            self.off = 0
        nel = 1
        for d_ in shape[1:]:
            nel *= d_
        esz = 4 if dt == F32 else 2
        words = (nel * esz + 3) // 4
        words = (words + 7) // 8 * 8
        assert self.off + words <= self.ARENA_WORDS, "SBUF arena overflow %s %d+%d" % (name, self.off, words)
        ap = self.arena[:, self.off:self.off + words]
        self.off += words
        self.peak = max(self.peak, self.off)
        if dt != F32:
            ap = ap.bitcast(dt)
        ap = ap[:, 0:nel]
        if len(shape) == 3:
            ap = ap.rearrange("p (a b) -> p a b", a=shape[1])
        elif len(shape) == 4:
            ap = ap.rearrange("p (a b c) -> p a b c", a=shape[1], b=shape[2])
        return ap

    def mark(self):
        return self.off

    def release(self, m):
        self.off = m
        barrier_all(self)

    def init_psum(self):
        for i in range(8):
            self.psum.append(self.stack.enter_context(self.nc.psum_tensor("ps%d" % i, [128, 512], F32)))

    def bank(self, cls, banks):
        i = self.psum_rr.get(cls, 0)
        self.psum_rr[cls] = i + 1
        b = banks[i % len(banks)]
        return self.psum[b], ("ps", b)

    def op(self, eng, fn, reads=(), writes=(), dma=False):
        need = {}
        for k in reads:
            w = self.lastw.get(k)
            if w:
                need[w[0]] = max(need.get(w[0], 0), w[1])
        for k in writes:
            w = self.lastw.get(k)
            if w:
                need[w[0]] = max(need.get(w[0], 0), w[1])
            for r in self.readers.get(k, ()):
                need[r[0]] = max(need.get(r[0], 0), r[1])
        if dma:
            r = self.dma_rr.get(eng, 0)
            self.dma_rr[eng] = r + 1
            sem = "dq_%s_%d" % (eng, r % self.NDS)
            if self.count.get(sem, 0) > 0:
                need[sem] = max(need.get(sem, 0), self.count[sem])
        else:
            sem = eng
        inc = 16 if dma else 1
        self.count[sem] = self.count.get(sem, 0) + inc
        tok = (sem, self.count[sem])
        waits = []
        for sm, v in need.items():
            if sm == "pe" and eng == "pe":
                continue
            if self.known[eng].get(sm, 0) < v:
                waits.append((sm, v))
                self.known[eng][sm] = v
        self.ops[eng].append((waits, fn, sem, inc))
        for k in reads:
            self.readers.setdefault(k, []).append(tok)
        for k in writes:
            self.lastw[k] = tok
            self.readers[k] = []

    def dma(self, eng, out, in_, reads=(), writes=()):
        self.op(eng, lambda e: e.dma_start(out=out, in_=in_), reads, writes, dma=True)

    def finish(self):
        nc = self.nc
        sems = {}
        names = list(self.count.keys())
        for n in names:
            sems[n] = self.stack.enter_context(nc.semaphore("s_" + n))
        emap = {"pe": "tensor", "act": "scalar", "dve": "vector", "pool": "gpsimd", "sp": "sync"}
        with nc.Block() as block:
            for e in self.ENGS:
                ops = self.ops[e]
                final = (e == "sp")

                def body(eng, ops=ops, final=final):
                    for waits, fn, sem, inc in ops:
                        for sm, v in waits:
                            eng.wait_ge(sems[sm], v)
                        if fn is not None:
                            fn(eng).then_inc(sems[sem], inc)
                    if final:
                        for n in names:
                            eng.wait_ge(sems[n], self.count[n])
                getattr(block, emap[e])(body)
        self.stack.close()


def barrier_all(P):
    snap = dict(P.count)
    for e in P.ENGS:
        waits = []
        for sm, v in snap.items():
            if P.known[e].get(sm, 0) < v:
                waits.append((sm, v))
                P.known[e][sm] = v
        if waits:
            P.ops[e].append((waits, None, None, 0))


class Ctx:
    pass


GEN = [4, 5, 6, 7]


def setup_common(P, nc, dram):
    C = Ctx()
    C.P = P
    C.nc = nc
    C.ident_f = P.sb([128, 128], F32, "ident_f")
    C.ident_b = P.sb([128, 128], BF16, "ident_b")
    C.ones_b = P.sb([128, 128], BF16, "ones_b")
    P.dma("sp", C.ident_f, dram["ident"][:, :], writes=["ident_f"])
    P.op("dve", lambda e: e.tensor_copy(out=C.ident_b, in_=C.ident_f), ["ident_f"], ["ident_b"])
    P.op("dve", lambda e: e.memset(C.ones_b, 1.0), [], ["ones_b"])
    C.epsD = P.sb([128, 1], F32, "epsD")
    P.op("dve", lambda e: e.memset(C.epsD, EPS * D), [], ["epsD"])
    C.eps64 = P.sb([128, 1], F32, "epsblk")
    P.op("dve", lambda e: e.memset(C.eps64, EPS * 64), [], ["epsblk"])
    return C


def emit_adaln(C, dram, pfx):
    P = C.P
    cfm = P.sb([128, 8, 2], F32, "cfm")
    scb = P.sb([128, 8, 2], BF16, "scb")
    bm = P.sb([128, 48], F32, "bm")
    gm = P.sb([128, 8], F32, "gmix")
    gf = P.sb([128, 8], F32, "gffn")
    mod = P.sb([128, 48, 2], F32, "mod")
    G0 = P.sb([128, 8, 2], F32, "G0")
    G1 = P.sb([128, 8, 2], F32, "G1")
    P.dma("sp", cfm, dram["cfm"][:, :, :], writes=["cfm"])
    P.dma("sp", bm, dram[pfx + "b_mod"][:, :], writes=["bm"])
    P.dma("sp", gm, dram[pfx + "g_mix"][:, :], writes=["gmix"])
    P.dma("sp", gf, dram[pfx + "g_ffn"][:, :], writes=["gffn"])
    P.op("act", lambda e: e.activation(out=scb, in_=cfm, func=AF.Silu), ["cfm"], ["scb"])
    wm = dram[pfx + "w_mod"]
    ps, pk = P.bank("gen", GEN)
    m = P.mark()
    wbufs = [P.sb([128, 8, 1024], BF16, "wmod%d" % i) for i in range(2)]
    for blk in range(6):
        wb = wbufs[blk % 2]
        key = "wmod%d" % (blk % 2)
        P.dma("pool", wb, wm[:, blk * 1024:(blk + 1) * 1024].rearrange("(c p) n -> p c n", p=128), writes=[key])
        for jj in range(8):
            j = blk * 8 + jj
            for c in range(8):
                P.op("pe", lambda e, wb=wb, jj=jj, c=c, j=j: e.matmul(
                    ps[:, j * 2:j * 2 + 2], lhsT=wb[:, c, jj * 128:(jj + 1) * 128], rhs=scb[:, c, :],
                    start=(c == 0), stop=(c == 7)), [key, "scb"], [pk])
    for s in range(2):
        P.op("dve", lambda e, s=s: e.tensor_tensor(
            out=mod[:, :, s], in0=ps[:, 0:96].rearrange("p (j s) -> p j s", s=2)[:, :, s], in1=bm,
            op=ALU.add), [pk, "bm"], ["mod"])
    C.mod = mod
    C.G = {0: G0, 1: G1}
    for which, (gv, gk, sc0) in enumerate([(gm, "gmix", 8), (gf, "gffn", 32)]):
        Gt = C.G[which]
        gkey = "G%d" % which
        for s in range(2):
            P.op("dve", lambda e, s=s, Gt=Gt, sc0=sc0, gv=gv: e.scalar_tensor_tensor(
                out=Gt[:, :, s], in0=mod[:, sc0:sc0 + 8, s], scalar=1.0, in1=gv,
                op0=ALU.add, op1=ALU.mult), ["mod", gk, gkey], [gkey])
        P.op("dve", lambda e, Gt=Gt: e.tensor_scalar(
            out=Gt, in0=Gt, scalar1=float(math.sqrt(D)), scalar2=None, op0=ALU.mult), [gkey], [gkey])
    C.shift = {0: 0, 1: 24}
    C.gate = {0: 16, 1: 40}
    P.release(m)


def alloc_norm(C):
    P = C.P
    C.sq = P.sb([128, 8, 512], BF16, "nsq")
    C.rs = P.sb([128, 512], F32, "nrs")
    C.ntmp = [P.sb([128, 512], F32, "ntmp%d" % i) for i in range(3)]
    C.nctr = 0


def emit_norm_mod(C, xa, xkey, n, s, which, hT, hkey, hf32=None, hf32key=None):
    P = C.P
    sq, rs = C.sq, C.rs
    for c in range(8):
        P.op("act", lambda e, c=c: e.activation(out=sq[:, c, :n], in_=xa[:, c, :n], func=AF.Square),
             [xkey(c)], [("nsq", c)])
    ps, pk = P.bank("gen", GEN)
    for c in range(8):
        P.op("pe", lambda e, c=c: e.matmul(ps[:, :n], lhsT=C.ones_b, rhs=sq[:, c, :n],
                                          start=(c == 0), stop=(c == 7)), [("nsq", c), "ones_b"], [pk])
    P.op("act", lambda e: e.activation(out=rs[:, :n], in_=ps[:, :n], func=AF.Sqrt, bias=C.epsD[:, 0:1], scale=1.0),
         [pk, "epsD"], ["nrs"])
    P.op("dve", lambda e: e.reciprocal(out=rs[:, :n], in_=rs[:, :n]), ["nrs"], ["nrs"])
    G = C.G[which]
    sh0 = C.shift[which]
    for c in range(8):
        C.nctr += 1
        j = C.nctr % 3
        tmp = C.ntmp[j]
        tk = "ntmp%d" % j
        P.op("dve", lambda e, c=c, tmp=tmp: e.scalar_tensor_tensor(
            out=tmp[:, :n], in0=xa[:, c, :n], scalar=G[:, c, s:s + 1], in1=rs[:, :n],
            op0=ALU.mult, op1=ALU.mult), [xkey(c), "G%d" % which, "nrs"], [tk])
        P.op("act", lambda e, c=c, tmp=tmp: e.activation(
            out=hT[:, c, :n], in_=tmp[:, :n], func=AF.Identity, bias=C.mod[:, sh0 + c, s:s + 1], scale=1.0),
            [tk, "mod"], [(hkey, c)])
        if hf32 is not None:
            P.op("pool", lambda e, c=c, tmp=tmp: e.tensor_scalar(
                out=hf32[:, c, :n], in0=tmp[:, :n], scalar1=C.mod[:, sh0 + c, s:s + 1], scalar2=None,
                op0=ALU.add), [tk, "mod"], [(hf32key, c)])


def alloc_qk(C):
    P = C.P
    C.qraw = P.sb([128, 512], F32, "qraw")
    C.qsq = P.sb([128, 512], BF16, "qsq")
    C.qrs = P.sb([128, 512], F32, "qrs")
    C.qnb = P.sb([128, 512], BF16, "qnb")
    C.qt1 = P.sb([128, 512], F32, "qt1")
    C.qt2 = P.sb([128, 512], F32, "qt2")


def emit_qknorm_rope(C, ps, pk, n, npart, gain, gkey, blk, blkkey, eps_blk, out_ap, out_key, rope=None):
    P = C.P
    qraw, qsq, qrs, qnb, qt1, qt2 = C.qraw, C.qsq, C.qrs, C.qnb, C.qt1, C.qt2
    pp = slice(0, npart)
    P.op("act", lambda e: e.copy(out=qraw[pp, :n], in_=ps[pp, :n]), [pk], ["qraw"])
    P.op("act", lambda e: e.activation(out=qsq[pp, :n], in_=ps[pp, :n], func=AF.Square), [pk], ["qsq"])
    ps2, pk2 = P.bank("gen", GEN)
    P.op("pe", lambda e: e.matmul(ps2[pp, :n], lhsT=blk[pp, pp], rhs=qsq[pp, :n], start=True, stop=True),
         ["qsq", blkkey], [pk2])
    P.op("act", lambda e: e.activation(out=qrs[pp, :n], in_=ps2[pp, :n], func=AF.Sqrt, bias=eps_blk[pp, 0:1],
                                       scale=1.0), [pk2, "epsblk"], ["qrs"])
    P.op("dve", lambda e: e.reciprocal(out=qrs[pp, :n], in_=qrs[pp, :n]), ["qrs"], ["qrs"])
    if rope is None:
        P.op("dve", lambda e: e.scalar_tensor_tensor(
            out=out_ap, in0=qraw[pp, :n], scalar=gain[pp, 0:1], in1=qrs[pp, :n], op0=ALU.mult, op1=ALU.mult),
            ["qraw", gkey, "qrs"], [out_key])
        return
    perm_b, cos_ap, sin_ap, tabkey = rope
    P.op("dve", lambda e: e.scalar_tensor_tensor(
        out=qnb[pp, :n], in0=qraw[pp, :n], scalar=gain[pp, 0:1], in1=qrs[pp, :n], op0=ALU.mult, op1=ALU.mult),
        ["qraw", gkey, "qrs"], ["qnb"])
    ps3, pk3 = P.bank("gen", GEN)
    P.op("pe", lambda e: e.matmul(ps3[pp, :n], lhsT=perm_b[pp, pp], rhs=qnb[pp, :n], start=True, stop=True),
         ["qnb", "perm_b"], [pk3])
    P.op("pool", lambda e: e.tensor_tensor(out=qt1[pp, :n], in0=qnb[pp, :n], in1=cos_ap, op=ALU.mult),
         ["qnb", tabkey], ["qt1"])
    P.op("dve", lambda e: e.tensor_tensor(out=qt2[pp, :n], in0=ps3[pp, :n], in1=sin_ap, op=ALU.mult),
         [pk3, tabkey], ["qt2"])
    P.op("dve", lambda e: e.tensor_tensor(out=out_ap, in0=qt1[pp, :n], in1=qt2[pp, :n], op=ALU.add),
         ["qt1", "qt2"], [out_key])


def load_w_bf(C, dram_ap, rows, cols, name):
    P = C.P
    t = P.sb([128, rows // 128, cols], BF16, name)
    P.dma("pool", t, dram_ap.rearrange("(c p) n -> p c n", p=128), writes=[name])
    return t


def load_const_bf(C, dram_ap, shape, name):
    P = C.P
    t = P.sb(shape, BF16, name)
    P.dma("pool", t, dram_ap, writes=[name])
    return t


def load_small(C, dram_ap, shape, name, scale=None):
    P = C.P
    t = P.sb(shape, F32, name)
    P.dma("sp", t, dram_ap, writes=[name])
    if scale is not None:
        P.op("dve", lambda e: e.tensor_scalar(out=t, in0=t, scalar1=float(scale), scalar2=None, op0=ALU.mult),
             [name], [name])
    return t


class XStream:
    def __init__(self, C, dram):
        self.C = C
        self.d = dram
        self.slots = [C.P.sb([128, 8, 512], F32, "xg%d" % i) for i in range(2)]
        self.i = 0

    def load(self, grp, from_out=False):
        t0, n, s = grp
        P = self.C.P
        i = self.i % 2
        self.i += 1
        if from_out:
            src = self.d["octx"] if s == 1 else self.d["olat"]
        else:
            src = self.d["xctx"] if s == 1 else self.d["xlat"]
        o = 0 if s == 1 else t0 - TC
        xa = self.slots[i]
        P.dma("sp", xa[:, :, :n], src[:, :, o:o + n], reads=[("xd", t0)] if from_out else [],
              writes=[("xg", i, c) for c in range(8)])
        return xa, (lambda c, i=i: ("xg", i, c)), i

    def store(self, grp, i):
        t0, n, s = grp
        P = self.C.P
        dst = self.d["octx"] if s == 1 else self.d["olat"]
        o = 0 if s == 1 else t0 - TC
        P.dma("act", dst[:, :, o:o + n], self.slots[i][:, :, :n], reads=[("xg", i, c) for c in range(8)],
              writes=[("xd", t0)])


def alloc_attn(C, hw=1024):
    P = C.P
    C.pt = [P.sb([128, 512], BF16, "pt%d" % i) for i in range(3)]
    C.ptc = 0
    C.rden = [P.sb([128, 4], F32, "rden%d" % i) for i in range(2)]
    C.otok = P.sb([128, 4, hw], BF16, "otok")
    C.oT = P.sb([128, hw // 128, 512], BF16, "oT")
    C.octr = 0


def emit_attn_head(C, n, q_ap, qkey, kparts, vparts, ocol, extra=None, sub0=0, k2parts=None, q2_ap=None,
                   q2key=None):
    P = C.P
    nsub = n // 128
    po, pok = P.bank("O", [2, 3])
    nch = len(kparts)
    for kc in range(nch):
        k_ap, kkey = kparts[kc]
        v_ap, vkey = vparts[kc]
        ps, psk = P.bank("S", [0, 1])
        ex = extra[kc] if extra is not None else []
        two = k2parts is not None
        P.op("pe", lambda e, k_ap=k_ap, ps=ps, ex=ex, two=two: e.matmul(
            ps[:, :n], lhsT=k_ap, rhs=q_ap, start=True, stop=(len(ex) == 0 and not two)), [kkey, qkey], [psk])
        if two:
            k2_ap, k2key = k2parts[kc]
            P.op("pe", lambda e, k2_ap=k2_ap, ps=ps: e.matmul(ps[:, :n], lhsT=k2_ap, rhs=q2_ap, start=False, stop=True),
                 [k2key, q2key], [psk])
        for bi, (b_ap, bkey) in enumerate(ex):
            P.op("pe", lambda e, b_ap=b_ap, ps=ps, bi=bi, ex=ex: e.matmul(
                ps[:, :n], lhsT=b_ap, rhs=C.ident_b[:, :n], start=False, stop=(bi == len(ex) - 1)),
                [bkey, "ident_b"], [psk])
        i = C.ptc % 3
        C.ptc += 1
        pt = C.pt[i]
        ptk = "pt%d" % i
        P.op("act", lambda e, ps=ps, pt=pt: e.activation(out=pt[:, :n], in_=ps[:, :n], func=AF.Exp), [psk], [ptk])
        for sub in range(nsub):
            P.op("pe", lambda e, sub=sub, pt=pt, v_ap=v_ap, kc=kc: e.matmul(
                po[:, sub * 65:sub * 65 + 65], lhsT=pt[:, sub * 128:(sub + 1) * 128], rhs=v_ap,
                start=(kc == 0), stop=(kc == nch - 1)), [ptk, vkey], [pok])
    i = C.octr % 2
    C.octr += 1
    rd = C.rden[i]
    rdk = "rden%d" % i
    pov = po[:, 0:nsub * 65].rearrange("p (s d) -> p s d", d=65)
    P.op("dve", lambda e: e.reciprocal(out=rd[:, :nsub], in_=pov[:, :, 64]), [pok], [rdk])
    for sub in range(nsub):
        P.op("dve", lambda e, sub=sub: e.tensor_scalar(
            out=C.otok[:, sub0 + sub, ocol:ocol + 64], in0=po[:, sub * 65:sub * 65 + 64], scalar1=rd[:, sub:sub + 1],
            scalar2=None, op0=ALU.mult), [pok, rdk], [("otok", sub0 + sub, ocol // 128)])


def emit_oproj(C, n, s, xa, xkey, wo, wokey, nchunks=8):
    P = C.P
    nsub = n // 128
    for j in range(nchunks):
        ps, pk = P.bank("gen", GEN)
        psb = ps.bitcast(BF16)
        for sub in range(nsub):
            P.op("pe", lambda e, sub=sub, j=j, psb=psb: e.transpose(
                out=psb[:, sub * 128:(sub + 1) * 128], in_=C.otok[:, sub, j * 128:(j + 1) * 128],
                identity=C.ident_b), [("otok", sub, j), "ident_b"], [pk])
        P.op("act", lambda e, j=j, psb=psb: e.copy(out=C.oT[:, j, :n], in_=psb[:, :n]), [pk], [("oT", j)])
    g0 = C.gate[0]
    for oc in range(8):
        ps, pk = P.bank("gen", GEN)
        for j in range(nchunks):
            P.op("pe", lambda e, j=j, oc=oc, ps=ps: e.matmul(
                ps[:, :n], lhsT=wo[:, j, oc * 128:(oc + 1) * 128], rhs=C.oT[:, j, :n],
                start=(j == 0), stop=(j == nchunks - 1)), [wokey, ("oT", j)], [pk])
        P.op("dve", lambda e, oc=oc, ps=ps: e.scalar_tensor_tensor(
            out=xa[:, oc, :n], in0=ps[:, :n], scalar=C.mod[:, g0 + oc, s:s + 1], in1=xa[:, oc, :n],
            op0=ALU.mult, op1=ALU.add), [pk, "mod", xkey(oc)], [xkey(oc)])


def emit_ffn(C, dram, pfx, moe, need_ctx):
    P = C.P
    groups = [g for g in GROUPS if need_ctx or g[2] == 0]
    x = P.sb([128, 8, NT], F32, "x")
    h2 = P.sb([128, 8, NT], BF16, "h2")
    for (t0, n, s) in groups:
        src = dram["octx"] if s == 1 else dram["olat"]
        o = 0 if s == 1 else t0 - TC
        P.dma("sp", x[:, :, t0:t0 + n], src[:, :, o:o + n], reads=[("xd", t0)],
              writes=[("x", c, t0) for c in range(8)])
    g1 = C.gate[1]
    if moe:
        comb = P.sb([128, NT // 128, 8], F32, "comb")
        cw = P.sb([128, NT], F32, "cwb")
        combb = P.sb([128, 4, 128], F32, "combb")
    m = P.mark()
    alloc_norm(C)
    if moe:
        hf = P.sb([128, 8, 512], F32, "hf")
        wr = P.sb([128, 8, 8], F32, "wr")
        P.dma("sp", wr, dram[pfx + "w_router"][:, :, :], writes=["wr"])
        lg = P.sb([128, 8], F32, "lg")
        top8 = P.sb([128, 8], F32, "top8")
        negv1 = P.sb([128, 1], F32, "negv1")
        exl = P.sb([128, 8], F32, "exl")
        msk = P.sb([128, 8], F32, "msk")
        den = P.sb([128, 1], F32, "den")
    for (t0, n, s) in groups:
        xa = x[:, :, t0:t0 + n]
        xkey = lambda c, t0=t0: ("x", c, t0)
        hv = h2[:, :, t0:t0 + n]
        if not moe:
            emit_norm_mod(C, xa, xkey, n, s, 1, hv, ("h2", t0))
            continue
        emit_norm_mod(C, xa, xkey, n, s, 1, hv, ("h2", t0), hf, "hf")
        for sub in range(n // 128):
            ti = (t0 + sub * 128) // 128
            ps, pk = P.bank("gen", GEN)
            for c in range(8):
                P.op("pe", lambda e, c=c, sub=sub, ps=ps: e.matmul(
                    ps[:, 0:8], lhsT=hf[:, c, sub * 128:(sub + 1) * 128], rhs=wr[:, c, :],
                    start=(c == 0), stop=(c == 7)), [("hf", c), "wr"], [pk])
            P.op("act", lambda e, ps=ps: e.copy(out=lg, in_=ps[:, 0:8]), [pk], ["lg"])
            P.op("dve", lambda e: e.max(out=top8, in_=lg), ["lg"], ["top8"])
            P.op("dve", lambda e: e.tensor_scalar(out=negv1, in0=top8[:, 0:1], scalar1=-1.0, scalar2=None,
                                                  op0=ALU.mult), ["top8"], ["negv1"])
            P.op("act", lambda e: e.activation(out=exl, in_=lg, func=AF.Exp, bias=negv1[:, 0:1], scale=1.0),
                 ["lg", "negv1"], ["exl"])
            P.op("dve", lambda e: e.tensor_scalar(out=msk, in0=lg, scalar1=top8[:, 1:2], scalar2=None,
                                                  op0=ALU.is_ge), ["lg", "top8"], ["msk"])
            P.op("dve", lambda e: e.tensor_tensor(out=msk, in0=msk, in1=exl, op=ALU.mult), ["msk", "exl"], ["msk"])
            P.op("dve", lambda e: e.tensor_reduce(out=den, in_=msk, axis=AX.X, op=ALU.add), ["msk"], ["den"])
            P.op("dve", lambda e: e.reciprocal(out=den, in_=den), ["den"], ["den"])
            P.op("dve", lambda e, ti=ti: e.tensor_scalar(
                out=comb[:, ti, :], in0=msk, scalar1=den[:, 0:1], scalar2=None, op0=ALU.mult),
                ["msk", "den"], [("comb", ti)])
    P.release(m)
    if moe:
        blocks = [(ex, b, 4) for ex in range(NEXP) for b in range(D_FFE // 512)]
        ffw = D_FFE
    else:
        blocks = [(None, b, 4 if b < 5 else 2) for b in range(6)]
        ffw = D_FF
    wg = [P.sb([128, 8, 512], BF16, "wg%d" % i) for i in range(2)]
    wu = [P.sb([128, 8, 512], BF16, "wu%d" % i) for i in range(2)]
    wd = [P.sb([128, 4, 1024], BF16, "wd%d" % i) for i in range(2)]
    sg = [P.sb([128, 512], F32, "sg%d" % i) for i in range(2)]
    ab = [P.sb([128, 4, 512], BF16, "ab%d" % i) for i in range(2)]
    actr = 0
    sgc = 0
    cur_e = None
    for bi, (ex, b, nf) in enumerate(blocks):
        i = bi % 2
        if moe:
            wgu = dram[pfx + "we_gu"][ex]
            wdn = dram[pfx + "we_down"][ex]
        else:
            wgu = dram[pfx + "w_gu"]
            wdn = dram[pfx + "w_down"]
        c0 = b * 512
        P.dma("pool", wg[i][:, :, :nf * 128], wgu[:, c0:c0 + nf * 128].rearrange("(c p) n -> p c n", p=128),
              writes=["wg%d" % i])
        P.dma("pool", wu[i][:, :, :nf * 128],
              wgu[:, ffw + c0:ffw + c0 + nf * 128].rearrange("(c p) n -> p c n", p=128), writes=["wu%d" % i])
        P.dma("pool", wd[i][:, :nf, :], wdn[c0:c0 + nf * 128, :].rearrange("(f p) n -> p f n", p=128),
              writes=["wd%d" % i])
        if moe and ex != cur_e:
            cur_e = ex
            for (t0, n, s) in groups:
                ps, pk = P.bank("gen", GEN)
                for sub in range(n // 128):
                    ti = (t0 + sub * 128) // 128
                    P.op("dve", lambda e, ti=ti, ex=ex, sub=sub: e.tensor_copy(
                        out=combb[:, sub, :], in_=comb[:, ti, ex:ex + 1].to_broadcast([128, 128])),
                        [("comb", ti)], [("combb", sub)])
                    P.op("pe", lambda e, sub=sub, ps=ps: e.matmul(
                        ps[:, sub * 128:(sub + 1) * 128], lhsT=combb[:, sub, :], rhs=C.ident_f,
                        start=True, stop=True), [("combb", sub), "ident_f"], [pk])
                P.op("act", lambda e, ps=ps, t0=t0, n=n: e.copy(out=cw[:, t0:t0 + n], in_=ps[:, :n]),
                     [pk], [("cwb", t0)])
        for (t0, n, s) in groups:
            a = ab[actr % 2]
            ak = "ab%d" % (actr % 2)
            actr += 1
            hk = [(("h2", t0), c) for c in range(8)]
            for f in range(nf):
                psg, pkg = P.bank("gen", GEN)
                for c in range(8):
                    P.op("pe", lambda e, c=c, f=f, psg=psg, t0=t0, n=n, wgi=wg[i]: e.matmul(
                        psg[:, :n], lhsT=wgi[:, c, f * 128:(f + 1) * 128], rhs=h2[:, c, t0:t0 + n],
                        start=(c == 0), stop=(c == 7)), ["wg%d" % i, hk[c]], [pkg])
                psu, pku = P.bank("gen", GEN)
                for c in range(8):
                    P.op("pe", lambda e, c=c, f=f, psu=psu, t0=t0, n=n, wui=wu[i]: e.matmul(
                        psu[:, :n], lhsT=wui[:, c, f * 128:(f + 1) * 128], rhs=h2[:, c, t0:t0 + n],
                        start=(c == 0), stop=(c == 7)), ["wu%d" % i, hk[c]], [pku])
                sgt = sg[sgc % 2]
                sgk = "sg%d" % (sgc % 2)
                sgc += 1
                P.op("act", lambda e, psg=psg, sgt=sgt, n=n: e.activation(out=sgt[:, :n], in_=psg[:, :n], func=AF.Silu),
                     [pkg], [sgk])
                if moe:
                    P.op("pool", lambda e, sgt=sgt, t0=t0, n=n: e.tensor_tensor(
                        out=sgt[:, :n], in0=sgt[:, :n], in1=cw[:, t0:t0 + n], op=ALU.mult),
                        [sgk, ("cwb", t0)], [sgk])
                P.op("dve", lambda e, psu=psu, sgt=sgt, a=a, f=f, n=n: e.tensor_tensor(
                    out=a[:, f, :n], in0=sgt[:, :n], in1=psu[:, :n], op=ALU.mult), [sgk, pku], [(ak, f)])
            for oc in range(8):
                ps, pk = P.bank("gen", GEN)
                for f in range(nf):
                    P.op("pe", lambda e, f=f, oc=oc, ps=ps, a=a, n=n, wdi=wd[i], nf=nf: e.matmul(
                        ps[:, :n], lhsT=wdi[:, f, oc * 128:(oc + 1) * 128], rhs=a[:, f, :n],
                        start=(f == 0), stop=(f == nf - 1)), ["wd%d" % i, (ak, f)], [pk])
                P.op("dve", lambda e, oc=oc, ps=ps, t0=t0, n=n, s=s: e.scalar_tensor_tensor(
                    out=x[:, oc, t0:t0 + n], in0=ps[:, :n], scalar=C.mod[:, g1 + oc, s:s + 1],
                    in1=x[:, oc, t0:t0 + n], op0=ALU.mult, op1=ALU.add),
                    [pk, "mod", ("x", oc, t0)], [("x", oc, t0)])
    for (t0, n, s) in groups:
        dst = dram["octx"] if s == 1 else dram["olat"]
        o = 0 if s == 1 else t0 - TC
        P.dma("sp", dst[:, :, o:o + n], x[:, :, t0:t0 + n], reads=[("x", c, t0) for c in range(8)],
              writes=[("xd", t0)])


def dram_in(nc, name, shape, dt=F32):
    return nc.dram_tensor(name, list(shape), dt, kind="ExternalInput").ap()


def dram_out(nc, name, shape, dt=F32):
    return nc.dram_tensor(name, list(shape), dt, kind="ExternalOutput").ap()


def common_drams(nc, pfx):
    d = {}
    d["ident"] = dram_in(nc, "ident", [128, 128])
    d["cfm"] = dram_in(nc, "cfm", [128, 8, 2])
    d["xlat"] = dram_in(nc, "xlat", [128, 8, TL])
    d["xctx"] = dram_in(nc, "xctx", [128, 8, TC])
    d[pfx + "w_mod"] = dram_in(nc, pfx + "w_mod", [D, 6 * D])
    d[pfx + "b_mod"] = dram_in(nc, pfx + "b_mod", [128, 48])
    d[pfx + "g_mix"] = dram_in(nc, pfx + "g_mix", [128, 8])
    d[pfx + "g_ffn"] = dram_in(nc, pfx + "g_ffn", [128, 8])
    return d


def ffn_drams(nc, d, pfx, moe, need_ctx):
    if moe:
        d[pfx + "w_router"] = dram_in(nc, pfx + "w_router", [128, 8, 8])
        d[pfx + "we_gu"] = dram_in(nc, pfx + "we_gu", [NEXP, D, 2 * D_FFE])
        d[pfx + "we_down"] = dram_in(nc, pfx + "we_down", [NEXP, D_FFE, D])
    else:
        d[pfx + "w_gu"] = dram_in(nc, pfx + "w_gu", [D, 2 * D_FF])
        d[pfx + "w_down"] = dram_in(nc, pfx + "w_down", [D_FF, D])
    d["olat"] = dram_out(nc, "olat", [128, 8, TL])
    if need_ctx:
        d["octx"] = dram_out(nc, "octx", [128, 8, TC])


def rope_group_tabs(C, d, grp, cosb, sinb):
    P = C.P
    t0, n, s = grp
    o = t0 - TC
    P.dma("sp", cosb[:, :n], d["cos"][:, o:o + n], writes=["tabs"])
    P.dma("sp", sinb[:, :n], d["sin"][:, o:o + n], writes=["tabs"])


def build_gqa_kv(pfx):
    nc = bass.Bass("TRN2", target_bir_lowering=False)
    d = common_drams(nc, pfx)
    d["wk"] = dram_in(nc, "wk", [D, 256])
    d["wv"] = dram_in(nc, "wv", [D, 256])
    d["kgain"] = dram_in(nc, "kgain", [128, 1])
    d["blk"] = dram_in(nc, "blk", [128, 128])
    d["perm"] = dram_in(nc, "perm", [128, 128])
    d["cos"] = dram_in(nc, "cos", [128, TL])
    d["sin"] = dram_in(nc, "sin", [128, TL])
    d["kout"] = dram_out(nc, "kout", [128, 2, NT], BF16)
    d["vout"] = dram_out(nc, "vout", [128, NT // 128, 4, 65], BF16)
    P = Prog(nc)
    P.init_psum()
    C = setup_common(P, nc, d)
    emit_adaln(C, d, pfx)
    alloc_norm(C)
    alloc_qk(C)
    xs = XStream(C, d)
    wk = load_w_bf(C, d["wk"], D, 256, "wk")
    wv = load_w_bf(C, d["wv"], D, 256, "wv")
    kgain = load_small(C, d["kgain"][:, :], [128, 1], "kgain", scale=8.0)
    blk = load_const_bf(C, d["blk"][:, :], [128, 128], "blk_b")
    perm = load_const_bf(C, d["perm"][:, :], [128, 128], "perm_b")
    cosb = P.sb([128, 512], F32, "cosb")
    sinb = P.sb([128, 512], F32, "sinb")
    kT = P.sb([128, 2, NT], BF16, "kT")
    vS = P.sb([128, NT // 128, 4, 65], BF16, "vS")
    P.op("pool", lambda e: e.memset(vS, 1.0), [], ["vS"])
    hT = [P.sb([128, 8, 512], BF16, "hT%d" % i) for i in range(2)]
    for gi, grp in enumerate(GROUPS):
        t0, n, s = grp
        h = hT[gi % 2]
        hk = "hT%d" % (gi % 2)
        xa, xkey, xi = xs.load(grp)
        emit_norm_mod(C, xa, xkey, n, s, 0, h, hk)
        if s == 0:
            rope_group_tabs(C, d, grp, cosb, sinb)
        for kp in range(2):
            ps, pk = P.bank("gen", GEN)
            for c in range(8):
                P.op("pe", lambda e, c=c, kp=kp, ps=ps, h=h, n=n: e.matmul(
                    ps[:, :n], lhsT=wk[:, c, kp * 128:(kp + 1) * 128], rhs=h[:, c, :n],
                    start=(c == 0), stop=(c == 7)), ["wk", (hk, c)], [pk])
            rope = (perm, cosb[:, :n], sinb[:, :n], "tabs") if s == 0 else None
            emit_qknorm_rope(C, ps, pk, n, 128, kgain, "kgain", blk, "blk_b", C.eps64,
                             kT[:, kp, t0:t0 + n], ("kT", kp, t0), rope)
        for sub in range(n // 128):
            ti = (t0 + sub * 128) // 128
            ps, pk = P.bank("gen", GEN)
            for c in range(8):
                P.op("pe", lambda e, c=c, sub=sub, ps=ps, h=h: e.matmul(
                    ps[:, 0:256], lhsT=h[:, c, sub * 128:(sub + 1) * 128], rhs=wv[:, c, :],
                    start=(c == 0), stop=(c == 7)), ["wv", (hk, c)], [pk])
            P.op("act", lambda e, ti=ti, ps=ps: e.copy(
                out=vS[:, ti, :, 0:64], in_=ps[:, 0:256].rearrange("p (g d) -> p g d", d=64)),
                [pk, "vS"], [("vSt", ti)])
    P.dma("sp", d["kout"][:, :, :], kT, reads=[("kT", kp, g[0]) for kp in range(2) for g in GROUPS])
    P.dma("sp", d["vout"][:, :, :, :], vS, reads=[("vSt", ti) for ti in range(NT // 128)] + ["vS"])
    P.finish()
    return nc


def build_gqa_rest(pfx, moe, need_ctx):
    nc = bass.Bass("TRN2", target_bir_lowering=False)
    d = common_drams(nc, pfx)
    d["wq"] = dram_in(nc, "wq", [D, 1024])
    d["wo"] = dram_in(nc, "wo", [D, 1024])
    d["qgain"] = dram_in(nc, "qgain", [128, 1])
    d["blk"] = dram_in(nc, "blk", [128, 128])
    d["perm"] = dram_in(nc, "perm", [128, 128])
    d["cos"] = dram_in(nc, "cos", [128, TL])
    d["sin"] = dram_in(nc, "sin", [128, TL])
    NK = TC + 2 * TL
    d["kall"] = dram_in(nc, "kall", [128, 2, NK], BF16)
    d["vall"] = dram_in(nc, "vall", [128, NK // 128, 4, 65], BF16)
    ffn_drams(nc, d, pfx, moe, need_ctx)
    P = Prog(nc)
    P.init_psum()
    C = setup_common(P, nc, d)
    emit_adaln(C, d, pfx)
    m = P.mark()
    alloc_norm(C)
    alloc_qk(C)
    alloc_attn(C)
    xs = XStream(C, d)
    wq = load_w_bf(C, d["wq"], D, 1024, "wq")
    wo = load_w_bf(C, d["wo"], D, 1024, "wo")
    qgain = load_small(C, d["qgain"][:, :], [128, 1], "qgain")
    blk = load_const_bf(C, d["blk"][:, :], [128, 128], "blk_b")
    perm = load_const_bf(C, d["perm"][:, :], [128, 128], "perm_b")
    cosb = P.sb([128, 512], F32, "cosb")
    sinb = P.sb([128, 512], F32, "sinb")
    kall = P.sb([128, 2, NK], BF16, "kall")
    vall = P.sb([128, NK // 128, 4, 65], BF16, "vall")
    P.dma("sp", kall, d["kall"][:, :, :], writes=["kall"])
    P.dma("sp", vall, d["vall"][:, :, :, :], writes=["vall"])
    hT = P.sb([128, 8, 512], BF16, "hT")
    qT = P.sb([128, 8, 512], BF16, "qT")
    for gi, grp in enumerate(GROUPS):
        t0, n, s = grp
        if s == 1 and not need_ctx:
            continue
        xa, xkey, xi = xs.load(grp)
        emit_norm_mod(C, xa, xkey, n, s, 0, hT, "hT")
        if s == 0:
            rope_group_tabs(C, d, grp, cosb, sinb)
        for j in range(8):
            ps, pk = P.bank("gen", GEN)
            for c in range(8):
                P.op("pe", lambda e, c=c, j=j, ps=ps, n=n: e.matmul(
                    ps[:, :n], lhsT=wq[:, c, j * 128:(j + 1) * 128], rhs=hT[:, c, :n],
                    start=(c == 0), stop=(c == 7)), ["wq", ("hT", c)], [pk])
            rope = (perm, cosb[:, :n], sinb[:, :n], "tabs") if s == 0 else None
            emit_qknorm_rope(C, ps, pk, n, 128, qgain, "qgain", blk, "blk_b", C.eps64,
                             qT[:, j, :n], ("qT", j), rope)
        nkc = (TC // 128) if s == 1 else (NK // 128)
        for j in range(8):
            kp = j // 4
            for half in range(2):
                g = 2 * kp + half
                hs = slice(half * 64, half * 64 + 64)
                kparts = [(kall[hs, kp, kc * 128:(kc + 1) * 128], "kall") for kc in range(nkc)]
                vparts = [(vall[:, kc, g, :], "vall") for kc in range(nkc)]
                emit_attn_head(C, n, qT[hs, j, :n], ("qT", j), kparts, vparts, j * 128 + half * 64)
        emit_oproj(C, n, s, xa, xkey, wo, "wo")
        xs.store(grp, xi)
    P.release(m)
    if not SKIP_FFN:
        emit_ffn(C, d, pfx, moe, need_ctx)
    P.finish()
    return nc


NEXT = TC + 40 * 64


def build_nat_kv(pfx):
    nc = bass.Bass("TRN2", target_bir_lowering=False)
    d = common_drams(nc, pfx)
    d["wk"] = dram_in(nc, "wk", [D, 1024])
    d["wv"] = dram_in(nc, "wv", [D, 1024])
    d["kgain"] = dram_in(nc, "kgain", [128, 1])
    d["blk"] = dram_in(nc, "blk", [128, 128])
    d["kout"] = dram_out(nc, "kout", [128, 8, NT], BF16)
    d["vout"] = dram_out(nc, "vout", [128, NT // 128, 16, 65], BF16)
    P = Prog(nc)
    P.init_psum()
    C = setup_common(P, nc, d)
    emit_adaln(C, d, pfx)
    alloc_norm(C)
    alloc_qk(C)
    xs = XStream(C, d)
    wk = load_w_bf(C, d["wk"], D, 1024, "wk")
    wv = load_w_bf(C, d["wv"], D, 1024, "wv")
    kgain = load_small(C, d["kgain"][:, :], [128, 1], "kgain", scale=8.0)
    blk = load_const_bf(C, d["blk"][:, :], [128, 128], "blk_b")
    kg = [P.sb([128, 8, 512], BF16, "kg%d" % i) for i in range(2)]
    vg = [P.sb([128, 4, 16, 65], BF16, "vg%d" % i) for i in range(2)]
    for i in range(2):
        P.op("pool", lambda e, i=i: e.memset(vg[i], 1.0), [], ["vg%d" % i])
    hT = P.sb([128, 8, 512], BF16, "hT")
    for gi, grp in enumerate(GROUPS):
        t0, n, s = grp
        xa, xkey, xi = xs.load(grp)
        emit_norm_mod(C, xa, xkey, n, s, 0, hT, "hT")
        kb, kbk = kg[gi % 2], "kg%d" % (gi % 2)
        vb, vbk = vg[gi % 2], "vg%d" % (gi % 2)
        for j in range(8):
            ps, pk = P.bank("gen", GEN)
            for c in range(8):
                P.op("pe", lambda e, c=c, j=j, ps=ps, n=n: e.matmul(
                    ps[:, :n], lhsT=wk[:, c, j * 128:(j + 1) * 128], rhs=hT[:, c, :n],
                    start=(c == 0), stop=(c == 7)), ["wk", ("hT", c)], [pk])
            emit_qknorm_rope(C, ps, pk, n, 128, kgain, "kgain", blk, "blk_b", C.eps64,
                             kb[:, j, :n], (kbk, j), None)
        P.dma("act", d["kout"][:, :, t0:t0 + n], kb[:, :, :n], reads=[(kbk, j) for j in range(8)])
        for sub in range(n // 128):
            for hh in range(2):
                ps, pk = P.bank("gen", GEN)
                for c in range(8):
                    P.op("pe", lambda e, c=c, sub=sub, ps=ps, hh=hh: e.matmul(
                        ps[:, 0:512], lhsT=hT[:, c, sub * 128:(sub + 1) * 128], rhs=wv[:, c, hh * 512:(hh + 1) * 512],
                        start=(c == 0), stop=(c == 7)), ["wv", ("hT", c)], [pk])
                P.op("act", lambda e, sub=sub, ps=ps, hh=hh, vb=vb: e.copy(
                    out=vb[:, sub, hh * 8:(hh + 1) * 8, 0:64], in_=ps[:, 0:512].rearrange("p (g d) -> p g d", d=64)),
                    [pk, vbk], [(vbk, sub, hh)])
        ti0 = t0 // 128
        P.dma("act", d["vout"][:, ti0:ti0 + n // 128, :, :], vb[:, :n // 128, :, :],
              reads=[(vbk, sub, hh) for sub in range(n // 128) for hh in range(2)] + [vbk])
    P.finish()
    return nc


def build_nat_rest(pfx, moe, need_ctx):
    nc = bass.Bass("TRN2", target_bir_lowering=False)
    d = common_drams(nc, pfx)
    d["wq"] = dram_in(nc, "wq", [D, 1024])
    d["wo"] = dram_in(nc, "wo", [D, 1024])
    d["qgain"] = dram_in(nc, "qgain", [128, 1])
    d["blk"] = dram_in(nc, "blk", [128, 128])
    d["kext"] = dram_in(nc, "kext", [128, 8, NEXT], BF16)
    d["vext"] = dram_in(nc, "vext", [128, NEXT // 128, 16, 65], BF16)
    d["relb"] = dram_in(nc, "relb", [128, 16, 640])
    d["masks"] = dram_in(nc, "masks", [128, 5, 640])
    ffn_drams(nc, d, pfx, moe, need_ctx)
    P = Prog(nc)
    P.init_psum()
    C = setup_common(P, nc, d)
    emit_adaln(C, d, pfx)
    m = P.mark()
    alloc_norm(C)
    alloc_qk(C)
    alloc_attn(C)
    xs = XStream(C, d)
    wq = load_w_bf(C, d["wq"], D, 1024, "wq")
    wo = load_w_bf(C, d["wo"], D, 1024, "wo")
    qgain = load_small(C, d["qgain"][:, :], [128, 1], "qgain")
    blk = load_const_bf(C, d["blk"][:, :], [128, 128], "blk_b")
    masks = load_const_bf(C, d["masks"][:, :, :], [128, 5, 640], "masks")
    hT = P.sb([128, 8, 512], BF16, "hT")
    qT = P.sb([128, 8, 512], BF16, "qT")
    NCH = 10
    kb = [P.sb([128, NCH * 128], BF16, "nk%d" % i) for i in range(2)]
    vb = [P.sb([128, NCH, 2, 65], BF16, "nv%d" % i) for i in range(2)]
    bb = [P.sb([128, 2, 640], BF16, "nb%d" % i) for i in range(2)]
    ctr = 0
    for gi, grp in enumerate(GROUPS):
        t0, n, s = grp
        if s == 1 and not need_ctx:
            continue
        xa, xkey, xi = xs.load(grp)
        emit_norm_mod(C, xa, xkey, n, s, 0, hT, "hT")
        for j in range(8):
            ps, pk = P.bank("gen", GEN)
            for c in range(8):
                P.op("pe", lambda e, c=c, j=j, ps=ps, n=n: e.matmul(
                    ps[:, :n], lhsT=wq[:, c, j * 128:(j + 1) * 128], rhs=hT[:, c, :n],
                    start=(c == 0), stop=(c == 7)), ["wq", ("hT", c)], [pk])
            emit_qknorm_rope(C, ps, pk, n, 128, qgain, "qgain", blk, "blk_b", C.eps64,
                             qT[:, j, :n], ("qT", j), None)
        for j in range(8):
            i = ctr % 2
            ctr += 1
            k_, v_, b_ = kb[i], vb[i], bb[i]
            kk, vk, bk = "nk%d" % i, "nv%d" % i, "nb%d" % i
            P.dma("sp", k_[:, 0:256], d["kext"][:, j, 0:256], writes=[kk])
            P.dma("sp", v_[:, 0:2, :, :], d["vext"][:, 0:2, 2 * j:2 * j + 2, :], writes=[vk])
            if s == 0:
                g4 = gi - 1
                c0 = 4 * g4
                P.dma("sp", k_[:, 256:256 + 8 * 128], d["kext"][:, j, TC + c0 * 128:TC + (c0 + 8) * 128], writes=[kk])
                P.dma("sp", v_[:, 2:10, :, :], d["vext"][:, 2 + c0:2 + c0 + 8, 2 * j:2 * j + 2, :], writes=[vk])
                P.dma("pool", b_, d["relb"][:, 2 * j:2 * j + 2, :], writes=[bk])
            for half in range(2):
                hs = slice(half * 64, half * 64 + 64)
                ocol = j * 128 + half * 64
                if s == 1:
                    kparts = [(k_[hs, kc * 128:(kc + 1) * 128], kk) for kc in range(2)]
                    vparts = [(v_[:, kc, half, :], vk) for kc in range(2)]
                    emit_attn_head(C, n, qT[hs, j, :n], ("qT", j), kparts, vparts, ocol)
                    continue
                for bl in range(4):
                    jb = 4 * g4 + bl
                    mt = {0: 1, 1: 2, 14: 3, 15: 4}.get(jb, 0)
                    chunks = [0, 1] + [2 + bl + e_ for e_ in range(5)]
                    kparts = [(k_[hs, kc * 128:(kc + 1) * 128], kk) for kc in chunks]
                    vparts = [(v_[:, kc, half, :], vk) for kc in chunks]
                    extra = [[], []] + [[(b_[:, half, e_ * 128:(e_ + 1) * 128], bk),
                                         (masks[:, mt, e_ * 128:(e_ + 1) * 128], "masks")] for e_ in range(5)]
                    emit_attn_head(C, 128, qT[hs, j, bl * 128:(bl + 1) * 128], ("qT", j), kparts, vparts, ocol,
                                   extra=extra, sub0=bl)
        emit_oproj(C, n, s, xa, xkey, wo, "wo")
        xs.store(grp, xi)
    P.release(m)
    emit_ffn(C, d, pfx, moe, need_ctx)
    P.finish()
    return nc


def nat_layer(inp, li, xlat, xctx, moe, need_ctx):
    pfx = "l%d_" % li
    wqkv = inp[pfx + "w_qkv"]
    wq, wk, wv = (np.ascontiguousarray(wqkv[:, i * 1024:(i + 1) * 1024]) for i in range(3))
    blk = block_ones([64, 64])
    kg = np.ascontiguousarray(np.tile(inp[pfx + "k_gain"], 2).reshape(128, 1))
    qg = np.ascontiguousarray(np.tile(inp[pfx + "q_gain"], 2).reshape(128, 1))
    nc_kv = get_prog(("nat_kv", pfx), lambda: build_nat_kv(pfx))
    ins = []
    for core in range(8):
        m = common_inputs(inp, pfx, xlat, xctx, core)
        m.update({"wk": wk, "wv": wv, "kgain": kg, "blk": blk})
        ins.append(m)
    res = run(nc_kv, ins)
    if DEBUG is not None:
        DEBUG["kv%d" % li] = [{k: np.asarray(v) for k, v in r.items()} for r in res]
    rb = inp[pfx + "rel_bias"]
    qr = np.arange(2)[:, None, None, None]
    qc = np.arange(64)[None, :, None, None]
    kr = np.arange(10)[None, None, :, None]
    kc = np.arange(64)[None, None, None, :]
    ridx = np.broadcast_to(kr - 4 - qr + 7, (2, 64, 10, 64))
    cidx = np.broadcast_to(np.clip(kc - qc + 15, 0, 30), (2, 64, 10, 64))
    relb = rb[:, ridx, cidx]
    relb = np.ascontiguousarray(relb.reshape(16, 128, 640).transpose(1, 0, 2)).astype(np.float32)
    masks = []
    for half in range(2):
        mk = np.zeros((5, 128, 640), np.float32)
        for ty, jb in enumerate([5, 0, 1, 14, 15]):
            for qrl in range(2):
                r = 32 * half + 2 * jb + qrl
                r0 = min(max(r - 4, 0), 56)
                for krl in range(10):
                    kr_g = 32 * half + 2 * jb - 4 + krl
                    rok = (r0 <= kr_g < r0 + 8)
                    for qcc in range(64):
                        c0 = min(max(qcc - 8, 0), 48)
                        ok = np.zeros(64, bool)
                        if rok:
                            ok[c0:c0 + 16] = True
                        mk[ty, qrl * 64 + qcc, krl * 64:(krl + 1) * 64] = np.where(ok, 0.0, -30000.0)
        masks.append(np.ascontiguousarray(mk.transpose(1, 0, 2)))
    nc_rest = get_prog(("nat_rest", pfx, moe, need_ctx), lambda: build_nat_rest(pfx, moe, need_ctx))
    ins = []
    for core in range(8):
        half = core % 2
        p0, p1 = (core // 2) * 2, (core // 2) * 2 + 1
        k_own, v_own = np.asarray(res[core]["kout"]), np.asarray(res[core]["vout"])
        k0, k1 = np.asarray(res[p0]["kout"]), np.asarray(res[p1]["kout"])
        v0, v1 = np.asarray(res[p0]["vout"]), np.asarray(res[p1]["vout"])
        kext = np.zeros((128, 8, NEXT), k_own.dtype)
        vext = np.zeros((128, NEXT // 128, 16, 65), v_own.dtype)
        kext[:, :, :TC] = k_own[:, :, :TC]
        vext[:, :2] = v_own[:, :2]
        kext[:, :, TC + 256:TC + 256 + TL] = k_own[:, :, TC:]
        vext[:, 4:4 + 16] = v_own[:, 2:]
        kext[:, :, TC:TC + 256] = k0[:, :, TC + TL - 256:]
        vext[:, 2:4] = v0[:, 16:18]
        kext[:, :, TC + 256 + TL:] = k1[:, :, TC:TC + 256]
        vext[:, 20:22] = v1[:, 2:4]
        m = common_inputs(inp, pfx, xlat, xctx, core)
        m.update({"wq": wq, "wo": np.ascontiguousarray(inp[pfx + "w_o"]), "qgain": qg, "blk": blk,
                  "kext": kext, "vext": vext, "relb": relb, "masks": masks[half]})
        m.update(ffn_inputs(inp, pfx, moe))
        ins.append(m)
    res = run(nc_rest, ins)
    xlat2 = [np.asarray(r["olat"]) for r in res]
    xctx2 = [np.asarray(r["octx"]) for r in res] if need_ctx else xctx
    return xlat2, xctx2


def emit_lowrank_norm(C, hT, hkey, w, wkey, col0, nch, n, gain, gkey, eps_t, epskey, out_fn, craw, csq):
    P = C.P
    for ch in range(nch):
        ps, pk = P.bank("gen", GEN)
        for c in range(8):
            P.op("pe", lambda e, c=c, ch=ch, ps=ps: e.matmul(
                ps[:, :n], lhsT=w[:, c, col0 + ch * 128:col0 + (ch + 1) * 128], rhs=hT[:, c, :n],
                start=(c == 0), stop=(c == 7)), [wkey, (hkey, c)], [pk])
        P.op("act", lambda e, ch=ch, ps=ps: e.copy(out=craw[:, ch, :n], in_=ps[:, :n]), [pk], [("craw", ch)])
        P.op("act", lambda e, ch=ch, ps=ps: e.activation(out=csq[:, ch, :n], in_=ps[:, :n], func=AF.Square),
             [pk], [("csq", ch)])
    ps2, pk2 = P.bank("gen", GEN)
    for ch in range(nch):
        P.op("pe", lambda e, ch=ch: e.matmul(ps2[:, :n], lhsT=C.ones_b, rhs=csq[:, ch, :n],
                                            start=(ch == 0), stop=(ch == nch - 1)), [("csq", ch), "ones_b"], [pk2])
    rs = C.qrs
    P.op("act", lambda e: e.activation(out=rs[:, :n], in_=ps2[:, :n], func=AF.Sqrt, bias=eps_t[:, 0:1], scale=1.0),
         [pk2, epskey], ["qrs"])
    P.op("dve", lambda e: e.reciprocal(out=rs[:, :n], in_=rs[:, :n]), ["qrs"], ["qrs"])
    for ch in range(nch):
        oap, okey = out_fn(ch)
        P.op("dve", lambda e, ch=ch, oap=oap: e.scalar_tensor_tensor(
            out=oap, in0=craw[:, ch, :n], scalar=gain[:, ch:ch + 1], in1=rs[:, :n], op0=ALU.mult, op1=ALU.mult),
            [("craw", ch), gkey, "qrs"], [okey])


def build_mla_kv(pfx):
    nc = bass.Bass("TRN2", target_bir_lowering=False)
    d = common_drams(nc, pfx)
    d["wckv"] = dram_in(nc, "wckv", [D, 256])
    d["wkr4"] = dram_in(nc, "wkr4", [D, 128])
    d["wkn"] = dram_in(nc, "wkn", [256, 1024])
    d["wv"] = dram_in(nc, "wv", [256, 1024])
    d["gdkv"] = dram_in(nc, "gdkv", [128, 2])
    d["kgn"] = dram_in(nc, "kgn", [128, 1])
    d["kgr"] = dram_in(nc, "kgr", [128, 1])
    d["blk64"] = dram_in(nc, "blk64", [128, 128])
    d["blk32"] = dram_in(nc, "blk32", [128, 128])
    d["perm"] = dram_in(nc, "perm", [128, 128])
    d["cos"] = dram_in(nc, "cos", [128, TL])
    d["sin"] = dram_in(nc, "sin", [128, TL])
    d["knout"] = dram_out(nc, "knout", [128, 8, NT], BF16)
    d["krout"] = dram_out(nc, "krout", [128, NT], BF16)
    d["vout"] = dram_out(nc, "vout", [128, NT // 128, 16, 65], BF16)
    P = Prog(nc)
    P.init_psum()
    C = setup_common(P, nc, d)
    emit_adaln(C, d, pfx)
    alloc_norm(C)
    alloc_qk(C)
    xs = XStream(C, d)
    wckv = load_w_bf(C, d["wckv"], D, 256, "wckv")
    wkr4 = load_w_bf(C, d["wkr4"], D, 128, "wkr4")
    wkn = load_w_bf(C, d["wkn"], 256, 1024, "wkn")
    wv = load_w_bf(C, d["wv"], 256, 1024, "wv")
    gdkv = load_small(C, d["gdkv"][:, :], [128, 2], "gdkv", scale=16.0)
    kgn = load_small(C, d["kgn"][:, :], [128, 1], "kgn", scale=8.0)
    kgr = load_small(C, d["kgr"][:, :], [128, 1], "kgr", scale=math.sqrt(32.0))
    blk64 = load_const_bf(C, d["blk64"][:, :], [128, 128], "blk64")
    blk32 = load_const_bf(C, d["blk32"][:, :], [128, 128], "blk32")
    perm = load_const_bf(C, d["perm"][:, :], [128, 128], "perm_b")
    eps32 = P.sb([128, 1], F32, "eps32")
    P.op("dve", lambda e: e.memset(eps32, EPS * 32), [], ["eps32"])
    eps256 = P.sb([128, 1], F32, "eps256")
    P.op("dve", lambda e: e.memset(eps256, EPS * 256), [], ["eps256"])
    cosb = P.sb([128, 512], F32, "cosb")
    sinb = P.sb([128, 512], F32, "sinb")
    craw = P.sb([128, 2, 512], F32, "craw")
    csq = P.sb([128, 2, 512], BF16, "csq")
    ckvn = P.sb([128, 2, 512], BF16, "ckvn")
    kg = [P.sb([128, 8, 512], BF16, "kg%d" % i) for i in range(2)]
    krg = [P.sb([128, 512], BF16, "krg%d" % i) for i in range(2)]
    vg = [P.sb([128, 4, 16, 65], BF16, "vg%d" % i) for i in range(2)]
    for i in range(2):
        P.op("pool", lambda e, i=i: e.memset(vg[i], 1.0), [], ["vg%d" % i])
    hT = P.sb([128, 8, 512], BF16, "hT")
    for gi, grp in enumerate(GROUPS):
        t0, n, s = grp
        xa, xkey, xi = xs.load(grp)
        emit_norm_mod(C, xa, xkey, n, s, 0, hT, "hT")
        if s == 0:
            rope_group_tabs(C, d, grp, cosb, sinb)
        kb, kbk = kg[gi % 2], "kg%d" % (gi % 2)
        krb, krk = krg[gi % 2], "krg%d" % (gi % 2)
        vb, vbk = vg[gi % 2], "vg%d" % (gi % 2)
        emit_lowrank_norm(C, hT, "hT", wckv, "wckv", 0, 2, n, gdkv, "gdkv", eps256, "eps256",
                          lambda ch, n=n: (ckvn[:, ch, :n], ("ckvn", ch)), craw, csq)
        ps, pk = P.bank("gen", GEN)
        for c in range(8):
            P.op("pe", lambda e, c=c, ps=ps, n=n: e.matmul(ps[:, :n], lhsT=wkr4[:, c, :], rhs=hT[:, c, :n],
                                                         start=(c == 0), stop=(c == 7)), ["wkr4", ("hT", c)], [pk])
        rope = (perm, cosb[:, :n], sinb[:, :n], "tabs") if s == 0 else None
        emit_qknorm_rope(C, ps, pk, n, 128, kgr, "kgr", blk32, "blk32", eps32, krb[:, :n], krk, rope)
        P.dma("act", d["krout"][:, t0:t0 + n], krb[:, :n], reads=[krk])
        for j in range(8):
            ps, pk = P.bank("gen", GEN)
            for cc in range(2):
                P.op("pe", lambda e, cc=cc, j=j, ps=ps, n=n: e.matmul(
                    ps[:, :n], lhsT=wkn[:, cc, j * 128:(j + 1) * 128], rhs=ckvn[:, cc, :n],
                    start=(cc == 0), stop=(cc == 1)), ["wkn", ("ckvn", cc)], [pk])
            emit_qknorm_rope(C, ps, pk, n, 128, kgn, "kgn", blk64, "blk64", C.eps64, kb[:, j, :n], (kbk, j), None)
        P.dma("act", d["knout"][:, :, t0:t0 + n], kb[:, :, :n], reads=[(kbk, j) for j in range(8)])
        for sub in range(n // 128):
            for hh in range(2):
                ps, pk = P.bank("gen", GEN)
                for cc in range(2):
                    P.op("pe", lambda e, cc=cc, sub=sub, ps=ps, hh=hh: e.matmul(
                        ps[:, 0:512], lhsT=ckvn[:, cc, sub * 128:(sub + 1) * 128], rhs=wv[:, cc, hh * 512:(hh + 1) * 512],
                        start=(cc == 0), stop=(cc == 1)), ["wv", ("ckvn", cc)], [pk])
                P.op("act", lambda e, sub=sub, ps=ps, hh=hh, vb=vb: e.copy(
                    out=vb[:, sub, hh * 8:(hh + 1) * 8, 0:64], in_=ps[:, 0:512].rearrange("p (g d) -> p g d", d=64)),
                    [pk, vbk], [(vbk, sub, hh)])
        ti0 = t0 // 128
        P.dma("act", d["vout"][:, ti0:ti0 + n // 128, :, :], vb[:, :n // 128, :, :],
              reads=[(vbk, sub, hh) for sub in range(n // 128) for hh in range(2)] + [vbk])
    P.finish()
    return nc


def build_mla_rest(pfx, moe, need_ctx):
    nc = bass.Bass("TRN2", target_bir_lowering=False)
    d = common_drams(nc, pfx)
    NK = TC + 2 * TL
    d["wcq"] = dram_in(nc, "wcq", [D, 384])
    d["wqn"] = dram_in(nc, "wqn", [384, 1024])
    d["wqr"] = dram_in(nc, "wqr", [384, 512])
    d["wo"] = dram_in(nc, "wo", [D, 1024])
    d["gdq"] = dram_in(nc, "gdq", [128, 3])
    d["qgn"] = dram_in(nc, "qgn", [128, 1])
    d["qgr"] = dram_in(nc, "qgr", [128, 1])
    d["blk64"] = dram_in(nc, "blk64", [128, 128])
    d["blk32"] = dram_in(nc, "blk32", [128, 128])
    d["perm"] = dram_in(nc, "perm", [128, 128])
    d["cos"] = dram_in(nc, "cos", [128, TL])
    d["sin"] = dram_in(nc, "sin", [128, TL])
    d["knall"] = dram_in(nc, "knall", [4, 128, 2, NK], BF16)
    d["krall"] = dram_in(nc, "krall", [128, NK], BF16)
    d["vall"] = dram_in(nc, "vall", [4, 128, NK // 128, 4, 65], BF16)
    ffn_drams(nc, d, pfx, moe, need_ctx)
    P = Prog(nc)
    P.init_psum()
    C = setup_common(P, nc, d)
    emit_adaln(C, d, pfx)
    m = P.mark()
    alloc_norm(C)
    alloc_qk(C)
    alloc_attn(C, hw=256)
    xs = XStream(C, d)
    wcq = load_w_bf(C, d["wcq"], D, 384, "wcq")
    wqn = load_w_bf(C, d["wqn"], 384, 1024, "wqn")
    wqr = load_w_bf(C, d["wqr"], 384, 512, "wqr")
    wo = load_w_bf(C, d["wo"], D, 1024, "wo")
    gdq = load_small(C, d["gdq"][:, :], [128, 3], "gdq", scale=math.sqrt(384.0))
    qgn = load_small(C, d["qgn"][:, :], [128, 1], "qgn", scale=8.0 * 96 ** -0.5)
    qgr = load_small(C, d["qgr"][:, :], [128, 1], "qgr", scale=math.sqrt(32.0) * 96 ** -0.5)
    blk64 = load_const_bf(C, d["blk64"][:, :], [128, 128], "blk64")
    blk32 = load_const_bf(C, d["blk32"][:, :], [128, 128], "blk32")
    perm = load_const_bf(C, d["perm"][:, :], [128, 128], "perm_b")
    eps32 = P.sb([128, 1], F32, "eps32")
    P.op("dve", lambda e: e.memset(eps32, EPS * 32), [], ["eps32"])
    eps384 = P.sb([128, 1], F32, "eps384")
    P.op("dve", lambda e: e.memset(eps384, EPS * 384), [], ["eps384"])
    cosb = P.sb([128, 512], F32, "cosb")
    sinb = P.sb([128, 512], F32, "sinb")
    craw = P.sb([128, 3, 512], F32, "craw")
    csq = P.sb([128, 3, 512], BF16, "csq")
    cqn = P.sb([128, 3, NT], BF16, "cqn")
    hT = P.sb([128, 8, 512], BF16, "hT")
    krall = P.sb([128, NK], BF16, "krall")
    P.dma("sp", krall, d["krall"][:, :], writes=["krall"])
    knb = P.sb([128, 2, NK], BF16, "knb")
    vb = P.sb([128, NK // 128, 4, 65], BF16, "vb")
    qn = P.sb([128, 2, 512], BF16, "qn")
    qr = P.sb([128, 2, 512], BF16, "qr")
    groups = [g for g in GROUPS if need_ctx or g[2] == 0]
    for grp in groups:
        t0, n, s = grp
        xa, xkey, xi = xs.load(grp)
        emit_norm_mod(C, xa, xkey, n, s, 0, hT, "hT")
        emit_lowrank_norm(C, hT, "hT", wcq, "wcq", 0, 3, n, gdq, "gdq", eps384, "eps384",
                          lambda ch, t0=t0, n=n: (cqn[:, ch, t0:t0 + n], ("cqn", ch, t0)), craw, csq)
    for hg in range(4):
        P.dma("sp", knb, d["knall"][hg], writes=["knb"])
        P.dma("sp", vb, d["vall"][hg], writes=["vb"])
        for grp in groups:
            t0, n, s = grp
            xa, xkey, xi = xs.load(grp, from_out=(hg > 0))
            if s == 0:
                rope_group_tabs(C, d, grp, cosb, sinb)
            for jj in range(2):
                j = hg * 2 + jj
                ps, pk = P.bank("gen", GEN)
                for ch in range(3):
                    P.op("pe", lambda e, ch=ch, j=j, ps=ps, t0=t0, n=n: e.matmul(
                        ps[:, :n], lhsT=wqn[:, ch, j * 128:(j + 1) * 128], rhs=cqn[:, ch, t0:t0 + n],
                        start=(ch == 0), stop=(ch == 2)), ["wqn", ("cqn", ch, t0)], [pk])
                emit_qknorm_rope(C, ps, pk, n, 128, qgn, "qgn", blk64, "blk64", C.eps64, qn[:, jj, :n], ("qn", jj), None)
            for hp in range(2):
                ps, pk = P.bank("gen", GEN)
                col = (hg * 4 + hp * 2) * 32
                for ch in range(3):
                    P.op("pe", lambda e, ch=ch, col=col, ps=ps, t0=t0, n=n: e.matmul(
                        ps[0:64, :n], lhsT=wqr[:, ch, col:col + 64], rhs=cqn[:, ch, t0:t0 + n],
                        start=(ch == 0), stop=(ch == 2)), ["wqr", ("cqn", ch, t0)], [pk])
                rope = (perm, cosb[0:64, :n], sinb[0:64, :n], "tabs") if s == 0 else None
                emit_qknorm_rope(C, ps, pk, n, 64, qgr, "qgr", blk32, "blk32", eps32, qr[0:64, hp, :n], ("qr", hp), rope)
            nkc = (TC // 128) if s == 1 else (NK // 128)
            for hl in range(4):
                jj, half = hl // 2, hl % 2
                hs = slice(half * 64, half * 64 + 64)
                rsl = slice((hl % 2) * 32, (hl % 2) * 32 + 32)
                kparts = [(knb[hs, jj, kc * 128:(kc + 1) * 128], "knb") for kc in range(nkc)]
                k2parts = [(krall[rsl, kc * 128:(kc + 1) * 128], "krall") for kc in range(nkc)]
                vparts = [(vb[:, kc, hl, :], "vb") for kc in range(nkc)]
                emit_attn_head(C, n, qn[hs, jj, :n], ("qn", jj), kparts, vparts, hl * 64,
                               k2parts=k2parts, q2_ap=qr[rsl, hl // 2, :n], q2key=("qr", hl // 2))
            emit_oproj(C, n, s, xa, xkey, wo[:, 2 * hg:2 * hg + 2, :], "wo", nchunks=2)
            xs.store(grp, xi)
    P.release(m)
    if not SKIP_FFN:
        emit_ffn(C, d, pfx, moe, need_ctx)
    P.finish()
    return nc


def mla_layer(inp, li, xlat, xctx, moe, need_ctx):
    pfx = "l%d_" % li
    w_in = inp[pfx + "w_in"]
    wcq = np.ascontiguousarray(w_in[:, :384])
    wckv = np.ascontiguousarray(w_in[:, 384:640])
    wkr4 = np.ascontiguousarray(np.tile(w_in[:, 640:672], (1, 4)))
    wuq = inp[pfx + "w_uq"].reshape(384, 16, 96)
    wqn = np.ascontiguousarray(wuq[:, :, :64].reshape(384, 1024))
    wqr = np.ascontiguousarray(wuq[:, :, 64:].reshape(384, 512))
    wukv = inp[pfx + "w_ukv"].reshape(256, 16, 128)
    wkn = np.ascontiguousarray(wukv[:, :, :64].reshape(256, 1024))
    wv = np.ascontiguousarray(wukv[:, :, 64:].reshape(256, 1024))
    blk64 = block_ones([64, 64])
    blk32 = block_ones([32, 32, 32, 32])
    perm = perm_matrix(16, 0, 32, 4)
    qg, kgv = inp[pfx + "q_gain"], inp[pfx + "k_gain"]
    col = lambda v: np.ascontiguousarray(v.reshape(128, 1).astype(np.float32))
    qgn, qgr = col(np.tile(qg[:64], 2)), col(np.tile(qg[64:], 4))
    kgn, kgr = col(np.tile(kgv[:64], 2)), col(np.tile(kgv[64:], 4))
    tabs = []
    for half in range(2):
        c, s_ = rope_tables(half, 16, 0, 32)
        tabs.append((np.tile(c, (4, 1)).astype(np.float32), np.tile(s_, (4, 1)).astype(np.float32)))
    nc_kv = get_prog(("mla_kv", pfx), lambda: build_mla_kv(pfx))
    ins = []
    for core in range(8):
        m = common_inputs(inp, pfx, xlat, xctx, core)
        m.update({"wckv": wckv, "wkr4": wkr4, "wkn": wkn, "wv": wv, "gdkv": vec_fm(inp[pfx + "g_dkv"], 2),
                  "kgn": kgn, "kgr": kgr, "blk64": blk64, "blk32": blk32, "perm": perm,
                  "cos": tabs[core % 2][0], "sin": tabs[core % 2][1]})
        ins.append(m)
    res = run(nc_kv, ins)
    if DEBUG is not None:
        DEBUG["kv%d" % li] = [{k: np.asarray(v) for k, v in r.items()} for r in res]
    nc_rest = get_prog(("mla_rest", pfx, moe, need_ctx), lambda: build_mla_rest(pfx, moe, need_ctx))
    ins = []
    for core in range(8):
        p0, p1 = (core // 2) * 2, (core // 2) * 2 + 1
        own = {k: np.asarray(v) for k, v in res[core].items()}
        r0 = {k: np.asarray(v) for k, v in res[p0].items()}
        r1 = {k: np.asarray(v) for k, v in res[p1].items()}
        kn = np.concatenate([own["knout"][:, :, :TC], r0["knout"][:, :, TC:], r1["knout"][:, :, TC:]], axis=2)
        kr = np.concatenate([own["krout"][:, :TC], r0["krout"][:, TC:], r1["krout"][:, TC:]], axis=1)
        vv = np.concatenate([own["vout"][:, :2], r0["vout"][:, 2:], r1["vout"][:, 2:]], axis=1)
        knall = np.ascontiguousarray(np.stack([kn[:, 2 * hg:2 * hg + 2] for hg in range(4)]))
        vall = np.ascontiguousarray(np.stack([vv[:, :, 4 * hg:4 * hg + 4] for hg in range(4)]))
        m = common_inputs(inp, pfx, xlat, xctx, core)
        m.update({"wcq": wcq, "wqn": wqn, "wqr": wqr, "wo": np.ascontiguousarray(inp[pfx + "w_o"]),
                  "gdq": vec_fm(inp[pfx + "g_dq"], 3), "qgn": qgn, "qgr": qgr,
                  "blk64": blk64, "blk32": blk32, "perm": perm,
                  "cos": tabs[core % 2][0], "sin": tabs[core % 2][1],
                  "knall": knall, "krall": np.ascontiguousarray(kr), "vall": vall})
        m.update(ffn_inputs(inp, pfx, moe))
        ins.append(m)
    res = run(nc_rest, ins)
    xlat2 = [np.asarray(r["olat"]) for r in res]
    xctx2 = [np.asarray(r["octx"]) for r in res] if need_ctx else xctx
    return xlat2, xctx2


def fm(a):
    T = a.shape[0]
    return np.ascontiguousarray(a.reshape(T, 8, 128).transpose(2, 1, 0))


def unfm(a):
    T = a.shape[2]
    return np.ascontiguousarray(a.transpose(2, 1, 0).reshape(T, 1024))


def vec_fm(v, nch):
    return np.ascontiguousarray(v.reshape(nch, 128).T)


def rope_tables(half, rot, lead, total):
    pos = np.arange(TL) + half * TL
    rows = (pos // 64).astype(np.float64)
    cols = (pos % 64).astype(np.float64)
    hw = rot // 2
    inv = np.exp(-math.log(10000.0) * np.arange(hw, dtype=np.float64) / hw)
    cos = np.ones((total, TL), np.float64)
    sin = np.zeros((total, TL), np.float64)
    for dd in range(2 * rot):
        axis = rows if dd < rot else cols
        w = dd % rot
        f = inv[w % hw]
        cos[lead + dd] = np.cos(axis * f)
        sin[lead + dd] = np.sin(axis * f) * (-1.0 if w < hw else 1.0)
    return cos, sin


def perm_matrix(rot, lead, total, nrep):
    M = np.zeros((128, 128), np.float32)
    hw = rot // 2
    for r in range(nrep):
        for dd in range(2 * rot):
            m = r * total + lead + dd
            w = dd % rot
            k = m + hw if w < hw else m - hw
            M[k, m] = 1.0
    return M


def block_ones(sizes):
    M = np.zeros((128, 128), np.float32)
    o = 0
    for sz in sizes:
        M[o:o + sz, o:o + sz] = 1.0
        o += sz
    return M


_prog_cache = {}


def get_prog(key, builder):
    if key not in _prog_cache:
        _prog_cache[key] = builder()
    return _prog_cache[key]


def run(nc, in_maps):
    res = run_bass_kernel_spmd(nc, in_maps, core_ids=list(range(8)))
    return res.results


def common_inputs(inp, pfx, xlat, xctx, core):
    b = core // 2
    cf = np.stack([inp["c"][b], inp["c_ctx"]], axis=-1)
    cfm = np.ascontiguousarray(cf.reshape(8, 128, 2).transpose(1, 0, 2))
    return {
        "ident": np.eye(128, dtype=np.float32),
        "cfm": cfm,
        "xlat": xlat[core], "xctx": xctx[core],
        pfx + "w_mod": inp[pfx + "w_mod"],
        pfx + "b_mod": vec_fm(inp[pfx + "b_mod"], 48),
        pfx + "g_mix": vec_fm(inp[pfx + "g_mix"], 8),
        pfx + "g_ffn": vec_fm(inp[pfx + "g_ffn"], 8),
    }


def ffn_inputs(inp, pfx, moe):
    if moe:
        wr = inp[pfx + "w_router"]
        return {pfx + "w_router": np.ascontiguousarray(wr.reshape(8, 128, 8).transpose(1, 0, 2)),
                pfx + "we_gu": inp[pfx + "we_gu"], pfx + "we_down": inp[pfx + "we_down"]}
    return {pfx + "w_gu": inp[pfx + "w_gu"], pfx + "w_down": inp[pfx + "w_down"]}


def gqa_layer(inp, li, xlat, xctx, moe, need_ctx):
    pfx = "l%d_" % li
    wqkv = inp[pfx + "w_qkv"]
    wq, wk, wv = wqkv[:, :1024], wqkv[:, 1024:1280], wqkv[:, 1280:1536]
    order = []
    for kp in range(2):
        for i in range(4):
            order += [8 * kp + i, 8 * kp + 4 + i]
    cols = np.concatenate([np.arange(h * 64, (h + 1) * 64) for h in order])
    wq_p = np.ascontiguousarray(wq[:, cols])
    wo_p = np.ascontiguousarray(inp[pfx + "w_o"][cols, :])
    blk = block_ones([64, 64])
    perm = perm_matrix(32, 0, 64, 2)
    kg = np.ascontiguousarray(np.tile(inp[pfx + "k_gain"], 2).reshape(128, 1))
    qg = np.ascontiguousarray(np.tile(inp[pfx + "q_gain"], 2).reshape(128, 1))
    tabs = []
    for half in range(2):
        c, s = rope_tables(half, 32, 0, 64)
        tabs.append((np.tile(c, (2, 1)).astype(np.float32), np.tile(s, (2, 1)).astype(np.float32)))
    nc_kv = get_prog(("gqa_kv", pfx), lambda: build_gqa_kv(pfx))
    ins = []
    for core in range(8):
        m = common_inputs(inp, pfx, xlat, xctx, core)
        m.update({"wk": np.ascontiguousarray(wk), "wv": np.ascontiguousarray(wv), "kgain": kg,
                  "blk": blk, "perm": perm, "cos": tabs[core % 2][0], "sin": tabs[core % 2][1]})
        ins.append(m)
    res = run(nc_kv, ins)
    if DEBUG is not None:
        DEBUG["kv%d" % li] = [{k: np.asarray(v) for k, v in r.items()} for r in res]
    nc_rest = get_prog(("gqa_rest", pfx, moe, need_ctx), lambda: build_gqa_rest(pfx, moe, need_ctx))
    ins = []
    for core in range(8):
        p0, p1 = (core // 2) * 2, (core // 2) * 2 + 1
        k_own = np.asarray(res[core]["kout"])
        v_own = np.asarray(res[core]["vout"])
        k0, k1 = np.asarray(res[p0]["kout"]), np.asarray(res[p1]["kout"])
        v0, v1 = np.asarray(res[p0]["vout"]), np.asarray(res[p1]["vout"])
        kall = np.concatenate([k_own[:, :, :TC], k0[:, :, TC:], k1[:, :, TC:]], axis=2)
        vall = np.concatenate([v_own[:, :TC // 128], v0[:, TC // 128:], v1[:, TC // 128:]], axis=1)
        m = common_inputs(inp, pfx, xlat, xctx, core)
        m.update({"wq": wq_p, "wo": wo_p, "qgain": qg, "blk": blk, "perm": perm,
                  "cos": tabs[core % 2][0], "sin": tabs[core % 2][1],
                  "kall": np.ascontiguousarray(kall), "vall": np.ascontiguousarray(vall)})
        m.update(ffn_inputs(inp, pfx, moe))
        ins.append(m)
    res = run(nc_rest, ins)
    xlat2 = [np.asarray(r["olat"]) for r in res]
    xctx2 = [np.asarray(r["octx"]) for r in res] if need_ctx else xctx
    return xlat2, xctx2


DEBUG = None
SKIP_FFN = False
NLAYERS = 4


def kernel(**inp):
    inp = {k: np.asarray(v) for k, v in inp.items()}
    x = inp["x"]
    ctx = inp["ctx"]
    xlat = [fm(x[c // 2, (c % 2) * TL:(c % 2 + 1) * TL]) for c in range(8)]
    xctx = [fm(ctx[c // 2]) for c in range(8)]
    for li in range(NLAYERS):
        kind = li % 3
        moe = (li % 2 == 1)
        need_ctx = li < 3
        if kind == 0:
            xlat, xctx = gqa_layer(inp, li, xlat, xctx, moe, need_ctx)
        elif kind == 1:
            xlat, xctx = nat_layer(inp, li, xlat, xctx, moe, need_ctx)
        else:
            xlat, xctx = mla_layer(inp, li, xlat, xctx, moe, need_ctx)
    out = np.zeros((4, 4096, 1024), np.float32)
    for c in range(8):
        out[c // 2, (c % 2) * TL:(c % 2 + 1) * TL] = unfm(xlat[c])
    if DEBUG is not None:
        DEBUG["xctx"] = [unfm(a) for a in xctx]
    return out
```
